# Optimizing a Trainium2 kernel written in Bass

```python
import jax, jax.numpy as jnp
from jax import lax
import numpy as np

D_MODEL = 1024
BATCH = 2
SEQ = 8192
DEPTH = 4

N_MIXERS = 2
HEAD_DIM = 64
MIX_WIDTH = D_MODEL
MEM_HEADS = 4
MEM_WIDTH = MEM_HEADS * HEAD_DIM
MIXER_WIDTH = MIX_WIDTH - MEM_WIDTH
SB_HEADS = MIXER_WIDTH // HEAD_DIM
CONV_WIDTH = 31
N_MEM = 256
N_EXPERTS = 32
TOP_K = 4
D_FF = D_MODEL
SWIGLU_LIMIT = 7.0
SWIGLU_ALPHA = 1.702
Q_BLOCK = 128
EXPERT_BLOCK = 128
LN_EPS = 1e-5
DN_ALPHA = (2 * DEPTH) ** 0.25
DN_BETA = (8 * DEPTH) ** -0.25
N_CONV_LAYERS = (DEPTH + 1) // 2
N_SB_LAYERS = DEPTH // 2

kernel_name = "hybrid_conv_stickbreak_memxattn_moe_deepnorm"


def layer_norm(x, g, b):
    xf = x.astype(jnp.float32)
    mu = jnp.mean(xf, axis=-1, keepdims=True)
    var = jnp.mean(jnp.square(xf - mu), axis=-1, keepdims=True)
    return ((xf - mu) * lax.rsqrt(var + LN_EPS) * g.astype(jnp.float32) + b.astype(jnp.float32)).astype(x.dtype)


def conformer_conv(u, dw_w, dw_b, ln_g, ln_b):
    a, gate = jnp.split(u, 2, axis=-1)
    h = a * jax.nn.sigmoid(gate)
    c = h.shape[-1]
    h = lax.conv_general_dilated(
        h, dw_w[:, None, :].astype(h.dtype), window_strides=(1,),
        padding=[(CONV_WIDTH - 1, 0)],
        dimension_numbers=('NWC', 'WIO', 'NWC'), feature_group_count=c) + dw_b
    return jax.nn.silu(layer_norm(h, ln_g, ln_b))


def stick_breaking_attention(q, k, v):
    b, s, h, dh = q.shape
    n_blk = s // Q_BLOCK
    scale = dh ** -0.5
    key_pos = jnp.arange(s)
    q_blocks = q.reshape(b, n_blk, Q_BLOCK, h, dh).transpose(1, 0, 2, 3, 4)

    def one_block(args):
        q_blk, blk = args
        q_pos = blk * Q_BLOCK + jnp.arange(Q_BLOCK)
        z = jnp.einsum('bqhd,bkhd->bhqk', q_blk, k, preferred_element_type=jnp.float32) * scale
        causal = key_pos[None, :] < q_pos[:, None]
        log_not = jnp.where(causal, jax.nn.log_sigmoid(-z), 0.0)
        tail = lax.cumsum(log_not, axis=3, reverse=True) - log_not
        w = jnp.where(causal, jnp.exp(jax.nn.log_sigmoid(z) + tail), 0.0)
        return jnp.einsum('bhqk,bkhd->bqhd', w.astype(v.dtype), v)

    out = lax.map(one_block, (q_blocks, jnp.arange(n_blk)))
    return out.transpose(1, 0, 2, 3, 4).reshape(b, s, h, dh)


def memory_attention(qm, mem_k, mem_v):
    s = jnp.einsum('bshd,bmhd->bhsm', qm, mem_k, preferred_element_type=jnp.float32) * (HEAD_DIM ** -0.5)
    p = jax.nn.softmax(s, axis=-1)
    return jnp.einsum('bhsm,bmhd->bshd', p.astype(mem_v.dtype), mem_v)


def moe_ffn(h, w_router, b_router, w_gate_up, b_gate_up, w_down, b_down):
    b, s, d = h.shape
    n_tok = b * s
    xt = h.reshape(n_tok, d)
    logits = jnp.matmul(xt, w_router, preferred_element_type=jnp.float32) + b_router.astype(jnp.float32)
    top_vals, top_idx = lax.top_k(logits, TOP_K)
    gates = jax.nn.softmax(top_vals, axis=-1)
    n_asg = n_tok * TOP_K
    expert_of = top_idx.reshape(n_asg).astype(jnp.int32)
    token_of = jnp.arange(n_asg, dtype=jnp.int32) // TOP_K
    order = jnp.argsort(expert_of, stable=True)
    sorted_e = expert_of[order]
    counts = jnp.bincount(expert_of, length=N_EXPERTS).astype(jnp.int32)
    starts = jnp.cumsum(counts) - counts
    padded = (counts + EXPERT_BLOCK - 1) // EXPERT_BLOCK * EXPERT_BLOCK
    pad_ends = jnp.cumsum(padded)
    pad_starts = pad_ends - padded
    dest = pad_starts[sorted_e] + jnp.arange(n_asg, dtype=jnp.int32) - starts[sorted_e]
    n_rows = n_asg + N_EXPERTS * EXPERT_BLOCK
    n_blocks = n_rows // EXPERT_BLOCK
    row_token = jnp.full((n_rows,), n_tok, jnp.int32).at[dest].set(token_of[order])
    row_gate = jnp.zeros((n_rows,), jnp.float32).at[dest].set(gates.reshape(n_asg)[order])
    block_expert = jnp.minimum(
        jnp.searchsorted(pad_ends, jnp.arange(n_blocks, dtype=jnp.int32) * EXPERT_BLOCK, side='right'),
        N_EXPERTS - 1).astype(jnp.int32)
    x_pad = jnp.concatenate([xt, jnp.zeros((1, d), xt.dtype)], axis=0)
    xs = x_pad[row_token].reshape(n_blocks, EXPERT_BLOCK, d)

    def run_block(args):
        xb, e = args
        gu = xb @ w_gate_up[e] + b_gate_up[e]
        gate = jnp.minimum(gu[:, :D_FF], SWIGLU_LIMIT)
        up = jnp.clip(gu[:, D_FF:], -SWIGLU_LIMIT, SWIGLU_LIMIT)
        act = (up + 1.0) * gate * jax.nn.sigmoid(SWIGLU_ALPHA * gate)
        return act @ w_down[e] + b_down[e]

    ys = lax.map(run_block, (xs, block_expert)).reshape(n_rows, d)
    ys = ys * row_gate[:, None].astype(ys.dtype)
    out = jnp.zeros((n_tok + 1, d), ys.dtype).at[row_token].add(ys)[:n_tok]
    return out.reshape(b, s, d)


def setup_inputs(seed: int = 0) -> dict:
    key = jax.random.key(seed)
    ks = jax.random.split(key, 24)
    f32 = jnp.float32
    d = D_MODEL

    def nrm(k, shape, scale):
        return jax.random.normal(k, shape, f32) * scale

    return {
        "x": nrm(ks[0], (BATCH, SEQ, d), 1.0),
        "mem": nrm(ks[1], (BATCH, N_MEM, d), 1.0),
        "mem_ln_g": 1.0 + nrm(ks[2], (d,), 0.02),
        "mem_ln_b": nrm(ks[3], (d,), 0.02),
        "w_mem_kv": nrm(ks[4], (d, 2 * MEM_WIDTH), d ** -0.5),
        "w_in_conv": nrm(ks[5], (N_CONV_LAYERS, d, 2 * MIXER_WIDTH + MEM_WIDTH), d ** -0.5),
        "conv_dw_w": nrm(ks[6], (N_CONV_LAYERS, CONV_WIDTH, MIXER_WIDTH), CONV_WIDTH ** -0.5),
        "conv_dw_b": nrm(ks[7], (N_CONV_LAYERS, MIXER_WIDTH), 0.02),
        "conv_ln_g": 1.0 + nrm(ks[8], (N_CONV_LAYERS, MIXER_WIDTH), 0.02),
        "conv_ln_b": nrm(ks[9], (N_CONV_LAYERS, MIXER_WIDTH), 0.02),
        "w_in_sb": nrm(ks[10], (N_SB_LAYERS, d, 3 * MIXER_WIDTH + MEM_WIDTH), d ** -0.5),
        "w_mix_out": nrm(ks[11], (DEPTH, MIX_WIDTH, d), DN_BETA * MIX_WIDTH ** -0.5),
        "ln_mix_g": 1.0 + nrm(ks[12], (DEPTH, d), 0.02),
        "ln_mix_b": nrm(ks[13], (DEPTH, d), 0.02),
        "w_router": nrm(ks[14], (DEPTH, d, N_EXPERTS), d ** -0.5),
        "b_router": nrm(ks[15], (DEPTH, N_EXPERTS), 0.01),
        "w_gate_up": nrm(ks[16], (DEPTH, N_EXPERTS, d, 2 * D_FF), d ** -0.5),
        "b_gate_up": nrm(ks[17], (DEPTH, N_EXPERTS, 2 * D_FF), 0.02),
        "w_down": nrm(ks[18], (DEPTH, N_EXPERTS, D_FF, d), DN_BETA * D_FF ** -0.5),
        "b_down": nrm(ks[19], (DEPTH, N_EXPERTS, d), 0.02),
        "ln_moe_g": 1.0 + nrm(ks[20], (DEPTH, d), 0.02),
        "ln_moe_b": nrm(ks[21], (DEPTH, d), 0.02),
    }


def reference(x, mem, mem_ln_g, mem_ln_b, w_mem_kv, w_in_conv, conv_dw_w, conv_dw_b, conv_ln_g,
              conv_ln_b, w_in_sb, w_mix_out, ln_mix_g, ln_mix_b, w_router, b_router, w_gate_up,
              b_gate_up, w_down, b_down, ln_moe_g, ln_moe_b):
    b, s, _ = x.shape
    mem_kv = layer_norm(mem, mem_ln_g, mem_ln_b) @ w_mem_kv
    mem_k = mem_kv[..., :MEM_WIDTH].reshape(b, -1, MEM_HEADS, HEAD_DIM)
    mem_v = mem_kv[..., MEM_WIDTH:].reshape(b, -1, MEM_HEADS, HEAD_DIM)

    h = x
    for i in range(DEPTH):
        j = i // N_MIXERS
        if i % N_MIXERS == 0:
            u = h @ w_in_conv[j]
            y_mix = conformer_conv(u[..., :2 * MIXER_WIDTH], conv_dw_w[j], conv_dw_b[j],
                                   conv_ln_g[j], conv_ln_b[j])
            qm = u[..., 2 * MIXER_WIDTH:]
        else:
            u = h @ w_in_sb[j]
            q = u[..., :MIXER_WIDTH].reshape(b, s, SB_HEADS, HEAD_DIM)
            k = u[..., MIXER_WIDTH:2 * MIXER_WIDTH].reshape(b, s, SB_HEADS, HEAD_DIM)
            v = u[..., 2 * MIXER_WIDTH:3 * MIXER_WIDTH].reshape(b, s, SB_HEADS, HEAD_DIM)
            y_mix = stick_breaking_attention(q, k, v).reshape(b, s, MIXER_WIDTH)
            qm = u[..., 3 * MIXER_WIDTH:]
        y_mem = memory_attention(qm.reshape(b, s, MEM_HEADS, HEAD_DIM), mem_k, mem_v).reshape(b, s, MEM_WIDTH)
        y = jnp.concatenate([y_mix, y_mem], axis=-1) @ w_mix_out[i]
        h = layer_norm(DN_ALPHA * h + y, ln_mix_g[i], ln_mix_b[i])
        y = moe_ffn(h, w_router[i], b_router[i], w_gate_up[i], b_gate_up[i], w_down[i], b_down[i])
        h = layer_norm(DN_ALPHA * h + y, ln_moe_g[i], ln_moe_b[i])
    return h
```

```python
import numpy as np
import ml_dtypes
import concourse.bass as bass
import concourse.mybir as mybir
from concourse.bass_utils import run_bass_kernel_spmd

F32 = mybir.dt.float32
BF16 = mybir.dt.bfloat16
I32 = mybir.dt.int32
AF = mybir.ActivationFunctionType
ALU = mybir.AluOpType

D = 1024
NCORES = 8
TPC = 2048
NT = TPC // 128
NE = 32
DFF = 1024
LN_EPS = 1e-5
DN_ALPHA = (2 * 4) ** 0.25
CW = 31
MIXW = 768
MEMW = 256
NMEM = 256


class Res:
    __slots__ = ("name", "w", "r", "sem", "dcount")

    def __init__(self, name):
        self.name = name
        self.w = None
        self.r = []
        self.sem = None
        self.dcount = 0


class Sync:
    ENGS = ("pe", "act", "dve", "pool", "sp")

    def __init__(self, nc, stack):
        self.nc = nc
        self.stack = stack
        self.eng = {"pe": nc.tensor, "act": nc.scalar, "dve": nc.vector, "pool": nc.gpsimd, "sp": nc.sync}
        self.sem = {}
        self.cnt = {}
        self.pending = {}
        for e in self.ENGS:
            self.sem[e] = stack.enter_context(nc.semaphore("s_" + e))
            self.cnt[e] = 0
            self.pending[e] = False
        self.seen = {e: {} for e in self.ENGS}
        self.nsem = 0

    def _new_sem(self, name):
        self.nsem += 1
        return self.stack.enter_context(self.nc.semaphore(name))

    def _wait(self, e, ticket):
        if ticket is None:
            return
        kind, key, val = ticket
        if kind == "eng" and key == e and e == "pe":
            return
        k = (kind, id(key) if kind == "dma" else key)
        if self.seen[e].get(k, 0) >= val:
            return
        self.seen[e][k] = val
        semobj = self.sem[key] if kind == "eng" else key
        self.eng[e].wait_ge(semobj, val)

    def deps(self, e, reads, writes):
        for r in reads:
            self._wait(e, r.w)
        for w in writes:
            self._wait(e, w.w)
            for t in w.r:
                self._wait(e, t)

    def op(self, e, fn, reads=(), writes=(), mark=True):
        self.deps(e, reads, writes)
        ins = fn()
        if mark:
            self.cnt[e] += 1
            ins.then_inc(self.sem[e], 1)
            self.pending[e] = False
            t = ("eng", e, self.cnt[e])
        else:
            self.pending[e] = True
            t = ("eng", e, self.cnt[e] + 1)
        for w in writes:
            w.w = t
            w.r = []
        for r in reads:
            r.r.append(t)
            if len(r.r) > 24:
                r.r = r.r[-24:] if False else r.r
        return ins

    def dma(self, q, out, in_, reads=(), writes=(), track=None):
        self.deps(q, reads, writes)
        tr = track if track is not None else writes[0]
        if tr.sem is None:
            tr.sem = self._new_sem("d_" + tr.name)
        ins = self.eng[q].dma_start(out=out, in_=in_)
        tr.dcount += 16
        ins.then_inc(tr.sem, 16)
        t = ("dma", tr.sem, tr.dcount)
        for w in writes:
            w.w = t
            w.r = []
        for r in reads:
            r.r.append(t)
        return ins

    def finish(self, final_res):
        for r in final_res:
            self._wait("sp", r.w)


def _mk(stack, nc, name, shape, dt):
    return stack.enter_context(nc.sbuf_tensor(name, shape, dt))


def _mkp(stack, nc, name, shape, dt=F32):
    return stack.enter_context(nc.psum_tensor(name, shape, dt))


def _consts(S, stack, nc):
    ident = _mk(stack, nc, "ident", [128, 128], F32)
    r_ident = Res("ident")
    S.op("pool", lambda: nc.gpsimd.memset(ident[:], 0.0), writes=[r_ident])
    S.op("pool", lambda: nc.gpsimd.affine_select(out=ident[:], in_=ident[:], pattern=[[-1, 128]],
                                                  compare_op=ALU.not_equal, fill=1.0, base=0,
                                                  channel_multiplier=1), reads=[r_ident], writes=[r_ident])
    return ident, r_ident


def _layernorm_tile(S, nc, r_t, r_res, stats, mv, rstd, gbc, bbc, r_stats, r_gb, out_t, r_out):
    for hf in range(2):
        S.op("dve", lambda hf=hf: nc.vector.bn_stats(out=stats[:, hf, :], in_=r_t[:, hf * 512:(hf + 1) * 512]),
             reads=[r_res], writes=[r_stats])
    S.op("dve", lambda: nc.vector.bn_aggr(out=mv[:], in_=stats[:].rearrange("p a b -> p (a b)")),
         reads=[r_stats], writes=[r_stats])
    S.op("act", lambda: nc.scalar.activation(out=rstd[:], in_=mv[:, 1:2], func=AF.Sqrt, bias=epsb_holder[0][:], scale=1.0),
         reads=[r_stats], writes=[r_stats])
    S.op("dve", lambda: nc.vector.reciprocal(out=rstd[:], in_=rstd[:]), reads=[r_stats], writes=[r_stats])
    S.op("dve", lambda: nc.vector.tensor_scalar(out=r_t[:], in0=r_t[:], scalar1=mv[:, 0:1], scalar2=rstd[:, 0:1],
                                                op0=ALU.subtract, op1=ALU.mult),
         reads=[r_res, r_stats], writes=[r_res])
    S.op("dve", lambda: nc.vector.tensor_tensor(out=r_t[:], in0=r_t[:], in1=gbc[:], op=ALU.mult),
         reads=[r_res, r_gb], writes=[r_res])
    S.op("dve", lambda: nc.vector.tensor_tensor(out=out_t[:], in0=r_t[:], in1=bbc[:], op=ALU.add),
         reads=[r_res, r_gb], writes=[r_out])


epsb_holder = [None]


def _eps_tile(S, stack, nc):
    epsb = _mk(stack, nc, "epsb", [128, 1], F32)
    r = Res("epsb")
    S.op("pool", lambda: nc.gpsimd.memset(epsb[:], LN_EPS), writes=[r])
    epsb_holder[0] = epsb
    epsb_holder.append(r)
    return epsb, r


def build_moe():
    from contextlib import ExitStack
    nc = bass.Bass("TRN2", target_bir_lowering=False)
    h_d = nc.dram_tensor("h", [TPC, D], F32, kind="ExternalInput").ap()
    wr_d = nc.dram_tensor("w_router", [D, NE], F32, kind="ExternalInput").ap()
    br_d = nc.dram_tensor("b_router", [1, NE], F32, kind="ExternalInput").ap()
    wgu_d = nc.dram_tensor("w_gate_up", [NE, D, 2 * DFF], F32, kind="ExternalInput").ap()
    bgu_d = nc.dram_tensor("b_gate_up", [NE, 2 * DFF], F32, kind="ExternalInput").ap()
    wd_d = nc.dram_tensor("w_down", [NE, DFF, D], F32, kind="ExternalInput").ap()
    bd_d = nc.dram_tensor("b_down", [NE, D], F32, kind="ExternalInput").ap()
    g_d = nc.dram_tensor("ln_g", [1, D], F32, kind="ExternalInput").ap()
    b_d = nc.dram_tensor("ln_b", [1, D], F32, kind="ExternalInput").ap()
    out_d = nc.dram_tensor("out", [TPC, D], F32, kind="ExternalOutput").ap()

    with ExitStack() as stack:
        S = Sync(nc, stack)
        V, A, P, T = nc.vector, nc.scalar, nc.gpsimd, nc.tensor
        ident, r_ident = _consts(S, stack, nc)
        epsb, r_eps = _eps_tile(S, stack, nc)

        hT = _mk(stack, nc, "hT", [128, 8, TPC], BF16)
        r_hT = [Res("hT%d" % i) for i in range(NT)]
        acc = _mk(stack, nc, "acc", [128, NT, D], F32)
        r_acc = [Res("acc%d" % i) for i in range(NT)]
        actT = _mk(stack, nc, "actT", [128, 8, TPC], BF16)
        r_actT = [[Res("actT%d_%d" % (fc, b)) for b in range(4)] for fc in range(8)]
        NWG = 2
        wgu = [_mk(stack, nc, "wgu%d" % i, [128, 8, 512], BF16) for i in range(NWG)]
        r_wgu = [Res("wgu%d" % i) for i in range(NWG)]
        wdb = [_mk(stack, nc, "wd%d" % i, [128, 8, 512], BF16) for i in range(2)]
        r_wd = [Res("wd%d" % i) for i in range(2)]
        gatew = _mk(stack, nc, "gatew", [128, NT, NE], F32)
        r_gw = [Res("gw%d" % i) for i in range(NT)]
        bgu = _mk(stack, nc, "bgu", [128, 16, NE], F32)
        r_bgu = Res("bgu")
        bd_sb = _mk(stack, nc, "bd_sb", [NE, D], F32)
        r_bd = Res("bd_sb")
        wr32 = _mk(stack, nc, "wr32", [128, 8, NE], F32)
        r_wr = Res("wr32")
        brbc = _mk(stack, nc, "brbc", [128, NE], F32)
        r_br = Res("brbc")
        gbc = _mk(stack, nc, "gbc", [128, D], F32)
        bbc = _mk(stack, nc, "bbc", [128, D], F32)
        r_gb = Res("gb")
        NW = 2
        htile = [_mk(stack, nc, "htile%d" % i, [128, D], F32) for i in range(NW)]
        r_ht = [Res("htile%d" % i) for i in range(NW)]
        hT32 = [_mk(stack, nc, "hT32_%d" % i, [128, 8, 128], F32) for i in range(NW)]
        r_hT32 = [Res("hT32_%d" % i) for i in range(NW)]
        wk = [[_mk(stack, nc, "wk%d_%d" % (i, j), [128, 512], F32) for j in range(3)] for i in range(2)]
        r_wk = [[Res("wk%d_%d" % (i, j)) for j in range(3)] for i in range(2)]
        small = [_mk(stack, nc, "small%d" % i, [128, 64], F32) for i in range(NW)]
        r_small = [Res("small%d" % i) for i in range(NW)]
        stats = [_mk(stack, nc, "stats%d" % i, [128, 2, 6], F32) for i in range(NW)]
        mv = [_mk(stack, nc, "mv%d" % i, [128, 2], F32) for i in range(NW)]
        rstd = [_mk(stack, nc, "rstd%d" % i, [128, 1], F32) for i in range(NW)]
        r_stats = [Res("stats%d" % i) for i in range(NW)]
        gwT = [_mk(stack, nc, "gwT%d" % i, [NE, 128], F32) for i in range(NW)]
        r_gwT = [Res("gwT%d" % i) for i in range(NW)]

        ps = [_mkp(stack, nc, "ps%d" % i, [128, 512]) for i in range(8)]
        r_ps = [Res("ps%d" % i) for i in range(8)]

        S.dma("sp", wr32[:], wr_d.rearrange("(c p) n -> p c n", p=128), writes=[r_wr])
        S.dma("sp", brbc[:], br_d[0:1, :].to_broadcast([128, NE]), writes=[r_br])
        S.dma("sp", gbc[:], g_d[0:1, :].to_broadcast([128, D]), writes=[r_gb])
        S.dma("sp", bbc[:], b_d[0:1, :].to_broadcast([128, D]), writes=[r_gb])
        S.dma("sp", bd_sb[:], bd_d[:, :], writes=[r_bd])
        for q in range(4):
            rows, r_rows = wk[0][q % 3], r_wk[0][q % 3]
            S.dma("sp", rows[:NE, :], bgu_d[:, q * 512:(q + 1) * 512], writes=[r_rows])
            for c4 in range(4):
                ch = q * 4 + c4
                pt = ps[ch % 2]
                S.op("pe", lambda c4=c4, pt=pt, rows=rows: T.transpose(pt[:, 0:NE], rows[:NE, c4 * 128:(c4 + 1) * 128], ident[:NE, :NE]),
                     reads=[r_rows, r_ident], writes=[r_ps[ch % 2]])
                S.op("dve", lambda ch=ch, pt=pt: V.tensor_copy(out=bgu[:, ch, :], in_=pt[:, 0:NE]),
                     reads=[r_ps[ch % 2]], writes=[r_bgu])

        for t in range(NT):
            w = t % NW
            S.dma("sp", htile[w][:], h_d[t * 128:(t + 1) * 128, :], writes=[r_ht[w]])
            for half in range(2):
                pb = 2 + half
                for c4 in range(4):
                    c = half * 4 + c4
                    S.op("pe", lambda c=c, c4=c4, pb=pb, w=w: T.transpose(ps[pb][:, c4 * 128:(c4 + 1) * 128],
                                                                       htile[w][:, c * 128:(c + 1) * 128], ident[:]),
                         reads=[r_ht[w], r_ident], writes=[r_ps[pb]], mark=(c4 == 3))
                S.op("dve", lambda half=half, pb=pb, t=t: V.tensor_copy(
                    out=hT[:, half * 4:(half + 1) * 4, t * 128:(t + 1) * 128],
                    in_=ps[pb][:].rearrange("p (c n) -> p c n", c=4)), reads=[r_ps[pb]], writes=[r_hT[t]])
                S.op("act", lambda half=half, pb=pb, w=w: A.copy(
                    out=hT32[w][:, half * 4:(half + 1) * 4, :],
                    in_=ps[pb][:].rearrange("p (c n) -> p c n", c=4)), reads=[r_ps[pb]], writes=[r_hT32[w]])
            pl = 4 + (t % 2)
            for c in range(8):
                S.op("pe", lambda c=c, pl=pl, w=w: T.matmul(ps[pl][:, 0:NE], hT32[w][:, c, :], wr32[:, c, :],
                                                         start=(c == 0), stop=(c == 7)),
                     reads=[r_hT32[w], r_wr], writes=[r_ps[pl]], mark=(c == 7))
            sm = small[w]
            S.op("dve", lambda pl=pl, sm=sm: V.tensor_tensor(out=sm[:, 0:NE], in0=ps[pl][:, 0:NE], in1=brbc[:], op=ALU.add),
                 reads=[r_ps[pl], r_br], writes=[r_small[w]])
            S.op("dve", lambda sm=sm: V.max(out=sm[:, 32:40], in_=sm[:, 0:NE]), reads=[r_small[w]], writes=[r_small[w]])
            S.op("dve", lambda sm=sm: V.tensor_scalar(out=sm[:, 40:41], in0=sm[:, 32:33], scalar1=-1.0, scalar2=None, op0=ALU.mult),
                 reads=[r_small[w]], writes=[r_small[w]])
            S.op("act", lambda sm=sm, t=t: A.activation(out=gatew[:, t, :], in_=sm[:, 0:NE], func=AF.Exp, bias=sm[:, 40:41], scale=1.0),
                 reads=[r_small[w]], writes=[r_gw[t]])
            S.op("dve", lambda sm=sm: V.tensor_scalar(out=sm[:, 0:NE], in0=sm[:, 0:NE], scalar1=sm[:, 35:36], scalar2=None, op0=ALU.is_ge),
                 reads=[r_small[w]], writes=[r_small[w]])
            S.op("dve", lambda sm=sm, t=t: V.tensor_tensor(out=gatew[:, t, :], in0=gatew[:, t, :], in1=sm[:, 0:NE], op=ALU.mult),
                 reads=[r_small[w], r_gw[t]], writes=[r_gw[t]])
            S.op("dve", lambda sm=sm, t=t: V.reduce_sum(out=sm[:, 41:42], in_=gatew[:, t, :], axis=mybir.AxisListType.X),
                 reads=[r_gw[t]], writes=[r_small[w]])
            S.op("dve", lambda sm=sm: V.reciprocal(out=sm[:, 42:43], in_=sm[:, 41:42]), reads=[r_small[w]], writes=[r_small[w]])
            S.op("dve", lambda sm=sm, t=t: V.tensor_scalar(out=gatew[:, t, :], in0=gatew[:, t, :], scalar1=sm[:, 42:43], scalar2=None, op0=ALU.mult),
                 reads=[r_small[w], r_gw[t]], writes=[r_gw[t]])

        piece_i = 0
        pp = 0
        for e in range(NE):
            for fp in range(4):
                wi = piece_i % NWG
                piece_i += 1
                src = wgu_d[e].rearrange("(c p) n -> p c n", p=128)
                S.dma("pool", wgu[wi][:, :, 0:256], src[:, :, fp * 256:(fp + 1) * 256], writes=[r_wgu[wi]])
                S.dma("pool", wgu[wi][:, :, 256:512], src[:, :, DFF + fp * 256:DFF + (fp + 1) * 256], writes=[r_wgu[wi]])
                for blk in range(4):
                    for j in range(2):
                        fc = fp * 2 + j
                        pg, pu = 2 * (pp % 2), 2 * (pp % 2) + 1
                        wks = wk[pp % 2]
                        rws = r_wk[pp % 2]
                        pp += 1
                        for c in range(8):
                            S.op("pe", lambda c=c, pg=pg, wi=wi, j=j, blk=blk: T.matmul(
                                ps[pg][:], wgu[wi][:, c, j * 128:(j + 1) * 128], hT[:, c, blk * 512:(blk + 1) * 512],
                                start=(c == 0), stop=(c == 7)),
                                 reads=[r_wgu[wi]] + r_hT[blk * 4:(blk + 1) * 4], writes=[r_ps[pg]], mark=(c == 7))
                        for c in range(8):
                            S.op("pe", lambda c=c, pu=pu, wi=wi, j=j, blk=blk: T.matmul(
                                ps[pu][:], wgu[wi][:, c, 256 + j * 128:256 + (j + 1) * 128], hT[:, c, blk * 512:(blk + 1) * 512],
                                start=(c == 0), stop=(c == 7)),
                                 reads=[r_wgu[wi]] + r_hT[blk * 4:(blk + 1) * 4], writes=[r_ps[pu]], mark=(c == 7))
                        gsb, sg, usb = wks
                        S.op("dve", lambda pg=pg, gsb=gsb, fc=fc, e=e: V.tensor_scalar(
                            out=gsb[:], in0=ps[pg][:], scalar1=bgu[:, fc, e:e + 1], scalar2=7.0, op0=ALU.add, op1=ALU.min),
                             reads=[r_ps[pg], r_bgu], writes=[rws[0]])
                        S.op("act", lambda gsb=gsb, sg=sg: A.activation(out=sg[:], in_=gsb[:], func=AF.Sigmoid, scale=1.702),
                             reads=[rws[0]], writes=[rws[1]])
                        S.op("dve", lambda pu=pu, usb=usb, fc=fc, e=e: V.tensor_scalar(
                            out=usb[:], in0=ps[pu][:], scalar1=bgu[:, 8 + fc, e:e + 1], scalar2=7.0, op0=ALU.add, op1=ALU.min),
                             reads=[r_ps[pu], r_bgu], writes=[rws[2]])
                        S.op("pool", lambda usb=usb: P.tensor_scalar(out=usb[:], in0=usb[:], scalar1=-7.0, scalar2=1.0,
                                                                     op0=ALU.max, op1=ALU.add),
                             reads=[rws[2]], writes=[rws[2]])
                        S.op("pool", lambda gsb=gsb, sg=sg: P.tensor_tensor(out=sg[:], in0=gsb[:], in1=sg[:], op=ALU.mult),
                             reads=[rws[0], rws[1]], writes=[rws[1]])
                        S.op("dve", lambda sg=sg, usb=usb, fc=fc, blk=blk: V.tensor_tensor(
                            out=actT[:, fc, blk * 512:(blk + 1) * 512], in0=sg[:], in1=usb[:], op=ALU.mult),
                             reads=[rws[1], rws[2]], writes=[r_actT[fc][blk]])
            srcd = wd_d[e].rearrange("(c p) n -> p c n", p=128)
            for half in range(2):
                S.dma("pool", wdb[half][:], srcd[:, :, half * 512:(half + 1) * 512], writes=[r_wd[half]])
            for t in range(NT):
                blk = t // 4
                for half in range(2):
                    py = 4 + 2 * (t % 2) + half
                    for fc in range(8):
                        S.op("pe", lambda fc=fc, py=py, t=t, half=half: T.matmul(
                            ps[py][:], actT[:, fc, t * 128:(t + 1) * 128], wdb[half][:, fc, :],
                            start=(fc == 0), stop=(fc == 7)),
                             reads=[r_actT[fc][blk], r_wd[half]], writes=[r_ps[py]], mark=(fc == 7))
                    if e == 0:
                        S.op("dve", lambda py=py, t=t, half=half, e=e: V.tensor_scalar(
                            out=acc[:, t, half * 512:(half + 1) * 512], in0=ps[py][:], scalar1=gatew[:, t, e:e + 1],
                            scalar2=None, op0=ALU.mult),
                             reads=[r_ps[py], r_gw[t]], writes=[r_acc[t]])
                    else:
                        S.op("dve", lambda py=py, t=t, half=half, e=e: V.scalar_tensor_tensor(
                            out=acc[:, t, half * 512:(half + 1) * 512], in0=ps[py][:], scalar=gatew[:, t, e:e + 1],
                            in1=acc[:, t, half * 512:(half + 1) * 512], op0=ALU.mult, op1=ALU.add),
                             reads=[r_ps[py], r_gw[t], r_acc[t]], writes=[r_acc[t]])

        r_out = [Res("outd%d" % i) for i in range(NT)]
        for t in range(NT):
            w = t % NW
            S.dma("sp", htile[w][:], h_d[t * 128:(t + 1) * 128, :], writes=[r_ht[w]])
            S.op("pe", lambda t=t: T.transpose(ps[0][:NE, 0:128], gatew[:, t, :], ident[:]),
                 reads=[r_gw[t], r_ident], writes=[r_ps[0]])
            S.op("dve", lambda w=w: V.tensor_copy(out=gwT[w][:], in_=ps[0][:NE, 0:128]), reads=[r_ps[0]], writes=[r_gwT[w]])
            for half in range(2):
                pb = 2 + half
                S.op("pe", lambda w=w, pb=pb, half=half: T.matmul(ps[pb][:], gwT[w][:], bd_sb[:, half * 512:(half + 1) * 512],
                                                               start=True, stop=True),
                     reads=[r_gwT[w], r_bd], writes=[r_ps[pb]])
                S.op("dve", lambda t=t, pb=pb, half=half: V.tensor_tensor(
                    out=acc[:, t, half * 512:(half + 1) * 512], in0=acc[:, t, half * 512:(half + 1) * 512], in1=ps[pb][:], op=ALU.add),
                     reads=[r_ps[pb], r_acc[t]], writes=[r_acc[t]])
            S.op("dve", lambda t=t, w=w: V.scalar_tensor_tensor(out=acc[:, t, :], in0=htile[w][:], scalar=float(DN_ALPHA),
                                                             in1=acc[:, t, :], op0=ALU.mult, op1=ALU.add),
                 reads=[r_ht[w], r_acc[t]], writes=[r_acc[t]])
            _layernorm_tile(S, nc, acc[:, t, :], r_acc[t], stats[w], mv[w], rstd[w], gbc, bbc, r_stats[w], r_gb,
                            htile[w], r_ht[w])
            S.dma("sp", out_d[t * 128:(t + 1) * 128, :], htile[w][:], reads=[r_ht[w]], writes=[r_out[t]])
        S.finish(r_out)
    return nc


def build_mix(kind):
    from contextlib import ExitStack
    nc = bass.Bass("TRN2", target_bir_lowering=False)
    WIN = 2 * MIXW + MEMW if kind == "conv" else 3 * MIXW + MEMW
    HALO = 32 if kind == "conv" else 0
    din = lambda n, s, dt=F32: nc.dram_tensor(n, s, dt, kind="ExternalInput").ap()
    h_d = din("h", [TPC, D])
    win_d = din("w_in", [D, WIN])
    if kind == "kv":
        kT_o = nc.dram_tensor("kT", [MIXW, TPC], BF16, kind="ExternalOutput").ap()
        v_o = nc.dram_tensor("v", [TPC, MIXW], BF16, kind="ExternalOutput").ap()
    else:
        mem_d = din("mem", [NMEM, D])
        mg_d = din("mem_ln_g", [1, D])
        mb_d = din("mem_ln_b", [1, D])
        wkv_d = din("w_mem_kv", [D, 2 * MEMW])
        wout_d = din("w_out", [D, D])
        g_d = din("ln_g", [1, D])
        b_d = din("ln_b", [1, D])
        out_d = nc.dram_tensor("out", [TPC, D], F32, kind="ExternalOutput").ap()
        if kind == "conv":
            halo_d = din("halo", [32, D])
            cvec_d = din("cvec", [34, MIXW])
        else:
            kTr_d = din("kT_rel", [4, MIXW, TPC], BF16)
            vr_d = din("v_rel", [4, TPC, MIXW], BF16)
            valid_d = din("valid", [1, 4])

    with ExitStack() as stack:
        S = Sync(nc, stack)
        V, A, P, T = nc.vector, nc.scalar, nc.gpsimd, nc.tensor
        ident, r_ident = _consts(S, stack, nc)
        epsb, r_eps = _eps_tile(S, stack, nc)
        oneb = _mk(stack, nc, "oneb", [128, 1], F32)
        r_c = Res("consts")
        S.op("pool", lambda: P.memset(oneb[:], 1.0), writes=[r_c])

        TW = HALO + TPC
        hT = _mk(stack, nc, "hT", [128, 8, TW], BF16)
        r_hT = [Res("hT%d" % i) for i in range(NT)]
        r_halo = Res("hThalo")
        wbig = _mk(stack, nc, "wbig", [128, 8, WIN], BF16)
        r_w = Res("wbig")
        htile = [_mk(stack, nc, "htile%d" % i, [128, D], F32) for i in range(2)]
        r_ht = [Res("htile%d" % i) for i in range(2)]
        ps = [_mkp(stack, nc, "ps%d" % i, [128, 512]) for i in range(8)]
        r_ps = [Res("ps%d" % i) for i in range(8)]

        S.dma("pool", wbig[:], win_d.rearrange("(c p) n -> p c n", p=128), writes=[r_w])

        def load_transpose(src_ap, nrows, col0, w, res):
            S.dma("sp", htile[w][:nrows, :], src_ap, writes=[r_ht[w]])
            for half in range(2):
                pb = 6 + half
                for c4 in range(4):
                    c = half * 4 + c4
                    S.op("pe", lambda c=c, c4=c4, pb=pb: T.transpose(ps[pb][:, c4 * 128:c4 * 128 + nrows],
                                                                   htile[w][:nrows, c * 128:(c + 1) * 128], ident[:nrows, :nrows]),
                         reads=[r_ht[w], r_ident], writes=[r_ps[pb]], mark=(c4 == 3))
                S.op("dve" if half == 0 else "act",
                     (lambda half=half, pb=pb: V.tensor_copy(
                         out=hT[:, half * 4:(half + 1) * 4, col0:col0 + nrows],
                         in_=ps[pb][:].rearrange("p (c n) -> p c n", c=4)[:, :, 0:nrows])) if half == 0 else
                     (lambda half=half, pb=pb: A.copy(
                         out=hT[:, half * 4:(half + 1) * 4, col0:col0 + nrows],
                         in_=ps[pb][:].rearrange("p (c n) -> p c n", c=4)[:, :, 0:nrows])),
                     reads=[r_ps[pb]], writes=[res])

        if kind == "conv":
            load_transpose(halo_d[:, :], 32, 0, 0, r_halo)
        for t in range(NT):
            load_transpose(h_d[t * 128:(t + 1) * 128, :], 128, HALO + t * 128, t % 2, r_hT[t])

        def inproj(pb, col_w, cols, ncols, reads):
            for c in range(8):
                S.op("pe", lambda c=c: T.matmul(ps[pb][:, 0:ncols], wbig[:, c, col_w:col_w + 128], hT[:, c, cols:cols + ncols],
                                                start=(c == 0), stop=(c == 7)),
                     reads=[r_w] + reads, writes=[r_ps[pb]], mark=(c == 7))

        if kind == "kv":
            stg = [_mk(stack, nc, "stg%d" % i, [128, MIXW], BF16) for i in range(2)]
            r_stg = [Res("stg%d" % i) for i in range(2)]
            r_o = []
            n = 0
            for blk in range(4):
                for kc in range(6):
                    pb = n % 2
                    n += 1
                    inproj(pb, MIXW + kc * 128, blk * 512, 512, r_hT[blk * 4:(blk + 1) * 4])
                    sb = stg[pb]
                    S.op("dve", lambda sb=sb, pb=pb: V.tensor_copy(out=sb[:, 0:512], in_=ps[pb][:]), reads=[r_ps[pb]], writes=[r_stg[pb]])
                    ro = Res("ko%d" % n)
                    S.dma("sp", kT_o[kc * 128:(kc + 1) * 128, blk * 512:(blk + 1) * 512], sb[:, 0:512], reads=[r_stg[pb]], writes=[ro])
                    r_o.append(ro)
            for t in range(NT):
                sb = stg[t % 2]
                for part, (c0, cn) in enumerate(((0, 512), (512, 256))):
                    pb = 2 + part
                    for c in range(8):
                        S.op("pe", lambda c=c, pb=pb, c0=c0, cn=cn, t=t: T.matmul(
                            ps[pb][:, 0:cn], hT[:, c, t * 128:(t + 1) * 128], wbig[:, c, 2 * MIXW + c0:2 * MIXW + c0 + cn],
                            start=(c == 0), stop=(c == 7)), reads=[r_w, r_hT[t]], writes=[r_ps[pb]], mark=(c == 7))
                    S.op("dve", lambda sb=sb, pb=pb, c0=c0, cn=cn: V.tensor_copy(out=sb[:, c0:c0 + cn], in_=ps[pb][:, 0:cn]),
                         reads=[r_ps[pb]], writes=[r_stg[t % 2]])
                ro = Res("vo%d" % t)
                S.dma("sp", v_o[t * 128:(t + 1) * 128, :], sb[:], reads=[r_stg[t % 2]], writes=[ro])
                r_o.append(ro)
            S.finish(r_o)
            return nc

        wkv = _mk(stack, nc, "wkv", [128, 8, 2 * MEMW], BF16)
        r_wkv = Res("wkv")
        S.dma("pool", wkv[:], wkv_d.rearrange("(c p) n -> p c n", p=128), writes=[r_wkv])
        gbc = _mk(stack, nc, "gbc", [128, D], F32)
        bbc = _mk(stack, nc, "bbc", [128, D], F32)
        r_gb = Res("gb")
        S.dma("sp", gbc[:], mg_d[0:1, :].to_broadcast([128, D]), writes=[r_gb])
        S.dma("sp", bbc[:], mb_d[0:1, :].to_broadcast([128, D]), writes=[r_gb])
        stats = [_mk(stack, nc, "stats%d" % i, [128, 2, 6], F32) for i in range(2)]
        mv = [_mk(stack, nc, "mv%d" % i, [128, 2], F32) for i in range(2)]
        rstd = [_mk(stack, nc, "rstd%d" % i, [128, 1], F32) for i in range(2)]
        r_stats = [Res("stats%d" % i) for i in range(2)]
        memT = _mk(stack, nc, "memT", [128, 8, NMEM], BF16)
        r_memT = Res("memT")
        wkf = [_mk(stack, nc, "wkf%d" % i, [128, D], F32) for i in range(2)]
        r_wkf = [Res("wkf%d" % i) for i in range(2)]
        for mt in range(2):
            S.dma("sp", htile[mt][:], mem_d[mt * 128:(mt + 1) * 128, :], writes=[r_ht[mt]])
            _layernorm_tile(S, nc, htile[mt], r_ht[mt], stats[mt], mv[mt], rstd[mt], gbc, bbc, r_stats[mt], r_gb, wkf[mt], r_wkf[mt])
            for half in range(2):
                pb = 6 + half
                for c4 in range(4):
                    c = half * 4 + c4
                    S.op("pe", lambda c=c, c4=c4, pb=pb, mt=mt: T.transpose(ps[pb][:, c4 * 128:(c4 + 1) * 128],
                                                                        wkf[mt][:, c * 128:(c + 1) * 128], ident[:]),
                         reads=[r_wkf[mt], r_ident], writes=[r_ps[pb]], mark=(c4 == 3))
                S.op("dve", lambda half=half, pb=pb, mt=mt: V.tensor_copy(
                    out=memT[:, half * 4:(half + 1) * 4, mt * 128:(mt + 1) * 128],
                    in_=ps[pb][:].rearrange("p (c n) -> p c n", c=4)), reads=[r_ps[pb]], writes=[r_memT])
        kTp = _mk(stack, nc, "kTp", [128, 4, NMEM], BF16)
        Vp = _mk(stack, nc, "Vp", [128, 2, 4, 128], BF16)
        onesp = _mk(stack, nc, "onesp", [128, 2, 128], BF16)
        r_mkv = Res("memkv")
        S.op("pool", lambda: P.memset(kTp[:], 0.0), writes=[r_mkv])
        S.op("pool", lambda: P.memset(Vp[:], 0.0), writes=[r_mkv])
        S.op("pool", lambda: P.memset(onesp[:], 0.0), writes=[r_mkv])
        S.op("pool", lambda: P.memset(onesp[:, 0, 0:64], 1.0), writes=[r_mkv])
        S.op("pool", lambda: P.memset(onesp[:, 1, 64:128], 1.0), writes=[r_mkv])
        for hp in range(2):
            for c in range(8):
                S.op("pe", lambda c=c, hp=hp: T.matmul(ps[0][:, 0:NMEM], wkv[:, c, hp * 128:(hp + 1) * 128], memT[:, c, :],
                                                       start=(c == 0), stop=(c == 7)),
                     reads=[r_wkv, r_memT], writes=[r_ps[0]], mark=(c == 7))
            for par in range(2):
                S.op("dve", lambda hp=hp, par=par: V.tensor_copy(out=kTp[par * 64:(par + 1) * 64, hp * 2 + par, :],
                                                                 in_=ps[0][par * 64:(par + 1) * 64, 0:NMEM]),
                     reads=[r_ps[0]], writes=[r_mkv])
        for mc in range(2):
            for c in range(8):
                S.op("pe", lambda c=c, mc=mc: T.matmul(ps[1][:, 0:MEMW], memT[:, c, mc * 128:(mc + 1) * 128], wkv[:, c, MEMW:2 * MEMW],
                                                       start=(c == 0), stop=(c == 7)),
                     reads=[r_wkv, r_memT], writes=[r_ps[1]], mark=(c == 7))
            for hh_ in range(4):
                par = hh_ % 2
                S.op("dve", lambda mc=mc, hh_=hh_, par=par: V.tensor_copy(out=Vp[:, mc, hh_, par * 64:(par + 1) * 64],
                                                                          in_=ps[1][:, hh_ * 64:(hh_ + 1) * 64]),
                     reads=[r_ps[1]], writes=[r_mkv])

        qmT = _mk(stack, nc, "qmT", [128, 2, TPC], BF16)
        r_qm = [Res("qm%d" % b) for b in range(4)]
        wk = [[_mk(stack, nc, "wk%d_%d" % (i, j), [128, 512], F32) for j in range(3)] for i in range(2)]
        r_wk = [[Res("wk%d_%d" % (i, j)) for j in range(3)] for i in range(2)]
        qm_col = 2 * MIXW if kind == "conv" else 3 * MIXW
        n = 0
        for blk in range(4):
            for j in range(2):
                pb = n % 2
                n += 1
                inproj(pb, qm_col + j * 128, HALO + blk * 512, 512, r_hT[blk * 4:(blk + 1) * 4])
                S.op("act", lambda pb=pb, j=j, blk=blk: A.copy(out=qmT[:, j, blk * 512:(blk + 1) * 512], in_=ps[pb][:]),
                     reads=[r_ps[pb]], writes=[r_qm[blk]])

        if kind == "conv":
            cw = _mk(stack, nc, "cw", [128, 6, 34], F32)
            r_cw = Res("cw")
            S.dma("sp", wkf[0][:34, 0:MIXW], cvec_d[:, :], writes=[r_wkf[0]])
            for cc in range(6):
                S.op("pe", lambda cc=cc: T.transpose(ps[2][:, 0:34], wkf[0][:34, cc * 128:(cc + 1) * 128], ident[:34, :34]),
                     reads=[r_wkf[0], r_ident], writes=[r_ps[2]])
                S.op("dve", lambda cc=cc: V.tensor_copy(out=cw[:, cc, :], in_=ps[2][:, 0:34]), reads=[r_ps[2]], writes=[r_cw])
            hh = [_mk(stack, nc, "hh%d" % i, [128, TW], F32) for i in range(2)]
            r_hh = [Res("hh%d" % i) for i in range(2)]
            co = _mk(stack, nc, "co", [128, 6, TPC], F32)
            r_co = [Res("co%d" % i) for i in range(6)]
            for cc in range(6):
                hb, r_hb = hh[cc % 2], r_hh[cc % 2]
                for bi in range(5):
                    cols, ncols = (0, 32) if bi == 0 else (HALO + (bi - 1) * 512, 512)
                    rd = [r_halo] if bi == 0 else r_hT[(bi - 1) * 4:bi * 4]
                    pa, pg = 2 * (n % 2), 2 * (n % 2) + 1
                    n += 1
                    inproj(pa, cc * 128, cols, ncols, rd)
                    inproj(pg, MIXW + cc * 128, cols, ncols, rd)
                    sg, r_sg = wk[n % 2][0], r_wk[n % 2][0]
                    S.op("act", lambda pg=pg, sg=sg, ncols=ncols: A.activation(out=sg[:, 0:ncols], in_=ps[pg][:, 0:ncols], func=AF.Sigmoid),
                         reads=[r_ps[pg]], writes=[r_sg])
                    S.op("dve", lambda pa=pa, sg=sg, hb=hb, cols=cols, ncols=ncols: V.tensor_tensor(
                        out=hb[:, cols:cols + ncols], in0=ps[pa][:, 0:ncols], in1=sg[:, 0:ncols], op=ALU.mult),
                         reads=[r_ps[pa], r_sg], writes=[r_hb])
                S.op("dve", lambda cc=cc, hb=hb: V.tensor_scalar(out=co[:, cc, :], in0=hb[:, 2:2 + TPC], scalar1=cw[:, cc, 0:1],
                                                                 scalar2=cw[:, cc, 31:32], op0=ALU.mult, op1=ALU.add),
                     reads=[r_hb, r_cw], writes=[r_co[cc]])
                for k in range(1, CW):
                    S.op("dve", lambda cc=cc, hb=hb, k=k: V.scalar_tensor_tensor(
                        out=co[:, cc, :], in0=hb[:, 2 + k:2 + k + TPC], scalar=cw[:, cc, k:k + 1], in1=co[:, cc, :],
                        op0=ALU.mult, op1=ALU.add), reads=[r_hb, r_cw, r_co[cc]], writes=[r_co[cc]])
            onesm = _mk(stack, nc, "onesm", [128, 128], F32)
            S.op("pool", lambda: P.memset(onesm[:], 1.0 / MIXW), writes=[r_c])
            for blk in range(4):
                bs = slice(blk * 512, (blk + 1) * 512)
                for cc in range(6):
                    S.op("pe", lambda cc=cc, bs=bs: T.matmul(ps[2][:], onesm[:], co[:, cc, bs], start=(cc == 0), stop=(cc == 5)),
                         reads=[r_c, r_co[cc]], writes=[r_ps[2]], mark=(cc == 5))
                for cc in range(6):
                    sq, r_sq = wk[cc % 2][1], r_wk[cc % 2][1]
                    S.op("act", lambda cc=cc, bs=bs, sq=sq: A.activation(out=sq[:], in_=co[:, cc, bs], func=AF.Square),
                         reads=[r_co[cc]], writes=[r_sq])
                    S.op("pe", lambda cc=cc, sq=sq: T.matmul(ps[3][:], onesm[:], sq[:], start=(cc == 0), stop=(cc == 5)),
                         reads=[r_c, r_sq], writes=[r_ps[3]])
                mean, r_mean = wk[0][2], r_wk[0][2]
                rs, r_rs = wk[1][2], r_wk[1][2]
                S.op("act", lambda mean=mean: A.copy(out=mean[:], in_=ps[2][:]), reads=[r_ps[2]], writes=[r_mean])
                S.op("dve", lambda mean=mean, rs=rs: V.tensor_tensor(out=rs[:], in0=mean[:], in1=mean[:], op=ALU.mult),
                     reads=[r_mean], writes=[r_rs])
                S.op("dve", lambda rs=rs: V.tensor_tensor(out=rs[:], in0=ps[3][:], in1=rs[:], op=ALU.subtract),
                     reads=[r_ps[3], r_rs], writes=[r_rs])
                S.op("act", lambda rs=rs: A.activation(out=rs[:], in_=rs[:], func=AF.Sqrt, bias=epsb[:], scale=1.0),
                     reads=[r_rs, r_eps], writes=[r_rs])
                S.op("dve", lambda rs=rs: V.reciprocal(out=rs[:], in_=rs[:]), reads=[r_rs], writes=[r_rs])
                for cc in range(6):
                    t1, r_t1 = wk[cc % 2][0], r_wk[cc % 2][0]
                    S.op("dve", lambda cc=cc, bs=bs, t1=t1, mean=mean: V.tensor_tensor(out=t1[:], in0=co[:, cc, bs], in1=mean[:], op=ALU.subtract),
                         reads=[r_co[cc], r_mean], writes=[r_t1])
                    S.op("pool", lambda t1=t1, rs=rs: P.tensor_tensor(out=t1[:], in0=t1[:], in1=rs[:], op=ALU.mult),
                         reads=[r_t1, r_rs], writes=[r_t1])
                    S.op("act", lambda cc=cc, t1=t1, blk=blk: A.activation(
                        out=hT[:, cc, HALO + blk * 512:HALO + (blk + 1) * 512], in_=t1[:], func=AF.Silu,
                        scale=cw[:, cc, 32:33], bias=cw[:, cc, 33:34]),
                         reads=[r_t1, r_cw], writes=r_hT[blk * 4:(blk + 1) * 4] + ([r_halo] if blk == 0 else []))
        else:
            _sb_attention(S, stack, nc, locals())

        PT = [_mk(stack, nc, "PT%d" % i, [128, 512], BF16) for i in range(4)]
        r_PT = [Res("PT%d" % i) for i in range(4)]
        n = 0
        for blk in range(4):
            for hp in range(2):
                for par in range(2):
                    hd = hp * 2 + par
                    for mc in range(2):
                        pb = 2 + (n % 2)
                        pt, r_pt = PT[n % 4], r_PT[n % 4]
                        n += 1
                        S.op("pe", lambda hd=hd, mc=mc, pb=pb, hp=hp, blk=blk: T.matmul(
                            ps[pb][:], kTp[:, hd, mc * 128:(mc + 1) * 128], qmT[:, hp, blk * 512:(blk + 1) * 512], start=True, stop=True),
                             reads=[r_mkv, r_qm[blk]], writes=[r_ps[pb]])
                        S.op("act", lambda pb=pb, pt=pt: A.activation(out=pt[:], in_=ps[pb][:], func=AF.Exp, scale=0.125),
                             reads=[r_ps[pb]], writes=[r_pt])
                        first, last = (par == 0 and mc == 0), (par == 1 and mc == 1)
                        S.op("pe", lambda hd=hd, mc=mc, pt=pt, first=first, last=last: T.matmul(
                            ps[4][:], Vp[:, mc, hd, :], pt[:], start=first, stop=last),
                             reads=[r_mkv, r_pt], writes=[r_ps[4]], mark=last)
                        S.op("pe", lambda par=par, pt=pt, first=first, last=last: T.matmul(
                            ps[5][:], onesp[:, par, :], pt[:], start=first, stop=last),
                             reads=[r_mkv, r_pt], writes=[r_ps[5]], mark=last)
                rd, r_rd = wk[hp][1], r_wk[hp][1]
                S.op("dve", lambda rd=rd: V.reciprocal(out=rd[:], in_=ps[5][:]), reads=[r_ps[5]], writes=[r_rd])
                S.op("dve", lambda rd=rd, hp=hp, blk=blk: V.tensor_tensor(
                    out=hT[:, 6 + hp, HALO + blk * 512:HALO + (blk + 1) * 512], in0=ps[4][:], in1=rd[:], op=ALU.mult),
                     reads=[r_ps[4], r_rd], writes=r_hT[blk * 4:(blk + 1) * 4])

        S.dma("pool", wbig[:, :, 0:D], wout_d.rearrange("(c p) n -> p c n", p=128), writes=[r_w])
        S.dma("sp", gbc[:], g_d[0:1, :].to_broadcast([128, D]), writes=[r_gb])
        S.dma("sp", bbc[:], b_d[0:1, :].to_broadcast([128, D]), writes=[r_gb])
        r_out = [Res("outd%d" % i) for i in range(NT)]
        for t in range(NT):
            w = t % 2
            S.dma("sp", htile[w][:], h_d[t * 128:(t + 1) * 128, :], writes=[r_ht[w]])
            for half in range(2):
                pb = 6 + half
                for c in range(8):
                    S.op("pe", lambda c=c, pb=pb, t=t, half=half: T.matmul(
                        ps[pb][:], hT[:, c, HALO + t * 128:HALO + (t + 1) * 128], wbig[:, c, half * 512:(half + 1) * 512],
                        start=(c == 0), stop=(c == 7)), reads=[r_hT[t], r_w], writes=[r_ps[pb]], mark=(c == 7))
                S.op("dve", lambda w=w, pb=pb, half=half: V.scalar_tensor_tensor(
                    out=wkf[w][:, half * 512:(half + 1) * 512], in0=htile[w][:, half * 512:(half + 1) * 512], scalar=float(DN_ALPHA),
                    in1=ps[pb][:], op0=ALU.mult, op1=ALU.add), reads=[r_ht[w], r_ps[pb]], writes=[r_wkf[w]])
            _layernorm_tile(S, nc, wkf[w], r_wkf[w], stats[w], mv[w], rstd[w], gbc, bbc, r_stats[w], r_gb, htile[w], r_ht[w])
            S.dma("sp", out_d[t * 128:(t + 1) * 128, :], htile[w][:], reads=[r_ht[w]], writes=[r_out[t]])
        S.finish(r_out)
    return nc


def _sb_attention(S, stack, nc, L):
    V, A, P, T = nc.vector, nc.scalar, nc.gpsimd, nc.tensor
    hT, r_hT, ps, r_ps, wk, r_wk, inproj = L["hT"], L["r_hT"], L["ps"], L["r_ps"], L["wk"], L["r_wk"], L["inproj"]
    kTr_d, vr_d, valid_d, oneb, r_c, r_w = L["kTr_d"], L["vr_d"], L["valid_d"], L["oneb"], L["r_c"], L["r_w"]
    qT = _mk(stack, nc, "qT", [128, 6, TPC], BF16)
    r_q = [Res("q%d" % b) for b in range(4)]
    n = 0
    for blk in range(4):
        for qc in range(6):
            pb = n % 2
            n += 1
            inproj(pb, qc * 128, blk * 512, 512, r_hT[blk * 4:(blk + 1) * 4])
            if n % 2:
                S.op("dve", lambda pb=pb, qc=qc, blk=blk: V.tensor_copy(out=qT[:, qc, blk * 512:(blk + 1) * 512], in_=ps[pb][:]),
                     reads=[r_ps[pb]], writes=[r_q[blk]])
            else:
                S.op("act", lambda pb=pb, qc=qc, blk=blk: A.copy(out=qT[:, qc, blk * 512:(blk + 1) * 512], in_=ps[pb][:]),
                     reads=[r_ps[pb]], writes=[r_q[blk]])
    U = _mk(stack, nc, "Umat", [128, 128], BF16)
    ones = _mk(stack, nc, "onesbf", [128, 128], BF16)
    masks = _mk(stack, nc, "masks", [128, 4, 512], BF16)
    vflag = _mk(stack, nc, "vflag", [128, 12], F32)
    r_k = Res("sbconst")
    S.op("pool", lambda: P.memset(ones[:], 1.0), writes=[r_k])
    S.op("pool", lambda: P.memset(U[:], 1.0), writes=[r_k])
    S.op("pool", lambda: P.affine_select(out=U[:], in_=U[:], pattern=[[-1, 128]], compare_op=ALU.is_gt, fill=0.0, base=0,
                                         channel_multiplier=1), reads=[r_k], writes=[r_k])
    S.op("pool", lambda: P.memset(masks[:], 1.0), writes=[r_k])
    for dd in range(4):
        S.op("pool", lambda dd=dd: P.affine_select(out=masks[:, dd, :], in_=masks[:, dd, :], pattern=[[1, 512]], compare_op=ALU.is_gt,
                                                   fill=0.0, base=-dd * 128, channel_multiplier=-1), reads=[r_k], writes=[r_k])
    S.dma("sp", vflag[:, 0:4], valid_d[0:1, :].to_broadcast([128, 4]), writes=[r_k])
    S.op("dve", lambda: V.tensor_scalar(out=vflag[:, 4:8], in0=vflag[:, 0:4], scalar1=-1.0, scalar2=None, op0=ALU.mult),
         reads=[r_k], writes=[r_k])
    S.op("dve", lambda: V.tensor_scalar(out=vflag[:, 8:12], in0=vflag[:, 0:4], scalar1=-1.0, scalar2=30000.0, op0=ALU.add, op1=ALU.mult),
         reads=[r_k], writes=[r_k])
    kpad = [[_mk(stack, nc, "kpad%d_%d" % (p_, b_), [128, TPC], BF16) for b_ in range(2)] for p_ in range(2)]
    vpad = [[_mk(stack, nc, "vpad%d_%d" % (p_, b_), [128, 16, 128], BF16) for b_ in range(2)] for p_ in range(2)]
    r_kv = [[Res("kv%d_%d" % (p_, b_)) for b_ in range(2)] for p_ in range(2)]
    for p_ in range(2):
        for b_ in range(2):
            S.op("pool", lambda p_=p_, b_=b_: P.memset(kpad[p_][b_][:], 0.0), writes=[r_kv[p_][b_]])
            S.op("pool", lambda p_=p_, b_=b_: P.memset(vpad[p_][b_][:], 0.0), writes=[r_kv[p_][b_]])
    Sacc = [_mk(stack, nc, "Sacc%d" % i, [128, 512], BF16) for i in range(4)]
    r_S = [Res("Sacc%d" % i) for i in range(4)]
    Lt = [_mk(stack, nc, "Lt%d" % i, [128, 512], BF16) for i in range(2)]
    Wt = [_mk(stack, nc, "Wt%d" % i, [128, 512], BF16) for i in range(2)]
    r_Lt = [Res("Lt%d" % i) for i in range(2)]
    r_Wt = [Res("Wt%d" % i) for i in range(2)]
    n = 0
    for hp in range(6):
        for par in range(2):
            hd = hp * 2 + par
            for rel in range(4):
                kb_, vb_, r_b = kpad[par][rel % 2], vpad[par][rel % 2], r_kv[par][rel % 2]
                S.dma("sp", kb_[par * 64:(par + 1) * 64, :], kTr_d[rel, hd * 64:(hd + 1) * 64, :], writes=[r_b])
                S.dma("sp", vb_[:, :, par * 64:(par + 1) * 64],
                      vr_d[rel, :, hd * 64:(hd + 1) * 64].rearrange("(kb p) d -> p kb d", p=128), writes=[r_b])
                for qb in range(4):
                    kbs = range(4 * qb + 3, -1, -1) if rel == 0 else range(15, -1, -1)
                    for kb in kbs:
                        first_sweep = (rel == 0 and kb == 4 * qb + 3)
                        o_first = first_sweep and par == 0
                        o_last = (par == 1 and rel == 3 and kb == 0)
                        dd = kb - 4 * qb if rel == 0 else -1
                        partial = dd >= 0
                        i2 = n % 2
                        n += 1
                        zb, tb, ob = i2, 2 + i2, 4 + qb
                        E, r_E = wk[i2][0], r_wk[i2][0]
                        Sp, r_Sp = wk[i2][1], r_wk[i2][1]
                        lt, r_lt, wt, r_wt = Lt[i2], r_Lt[i2], Wt[i2], r_Wt[i2]
                        qs = slice(qb * 512, (qb + 1) * 512)
                        S.op("pe", lambda zb=zb, kb_=kb_, kb=kb, hp=hp, qs=qs: T.matmul(
                            ps[zb][:], kb_[:, kb * 128:(kb + 1) * 128], qT[:, hp, qs], start=True, stop=True),
                             reads=[r_b, r_q[qb]], writes=[r_ps[zb]])
                        S.op("act", lambda zb=zb, E=E: A.activation(out=E[:], in_=ps[zb][:], func=AF.Exp, scale=0.125),
                             reads=[r_ps[zb]], writes=[r_E])
                        S.op("act", lambda E=E, Sp=Sp: A.activation(out=Sp[:], in_=E[:], func=AF.Ln, bias=oneb[:], scale=1.0),
                             reads=[r_E, r_c], writes=[r_Sp])
                        if partial:
                            S.op("dve", lambda Sp=Sp, lt=lt, rel=rel, dd=dd: V.scalar_tensor_tensor(
                                out=lt[:], in0=Sp[:], scalar=vflag[:, 4 + rel:5 + rel], in1=masks[:, dd, :], op0=ALU.mult, op1=ALU.mult),
                                 reads=[r_Sp, r_k], writes=[r_lt])
                        else:
                            S.op("dve", lambda Sp=Sp, lt=lt, rel=rel: V.tensor_scalar(
                                out=lt[:], in0=Sp[:], scalar1=vflag[:, 4 + rel:5 + rel], scalar2=None, op0=ALU.mult),
                                 reads=[r_Sp, r_k], writes=[r_lt])
                        S.op("pe", lambda tb=tb, lt=lt, fs=first_sweep: T.matmul(ps[tb][:], U[:], lt[:], start=True, stop=fs),
                             reads=[r_k, r_lt], writes=[r_ps[tb]], mark=first_sweep)
                        if not first_sweep:
                            S.op("pe", lambda tb=tb, qb=qb: T.matmul(ps[tb][:], ones[:], Sacc[qb][:], start=False, stop=True),
                                 reads=[r_k, r_S[qb]], writes=[r_ps[tb]])
                            S.op("pool", lambda qb=qb, lt=lt: P.tensor_tensor(out=Sacc[qb][:], in0=Sacc[qb][:], in1=lt[:], op=ALU.add),
                                 reads=[r_S[qb], r_lt], writes=[r_S[qb]])
                        else:
                            S.op("pool", lambda qb=qb, lt=lt: P.tensor_copy(out=Sacc[qb][:], in_=lt[:]),
                                 reads=[r_lt], writes=[r_S[qb]])
                        S.op("dve", lambda zb=zb, E=E, Sp=Sp: V.scalar_tensor_tensor(
                            out=E[:], in0=ps[zb][:], scalar=0.125, in1=Sp[:], op0=ALU.mult, op1=ALU.subtract),
                             reads=[r_ps[zb], r_Sp], writes=[r_E])
                        S.op("dve", lambda tb=tb, E=E: V.tensor_tensor(out=E[:], in0=E[:], in1=ps[tb][:], op=ALU.add),
                             reads=[r_E, r_ps[tb]], writes=[r_E])
                        S.op("act", lambda E=E, wt=wt, rel=rel: A.activation(out=wt[:], in_=E[:], func=AF.Exp,
                                                                           bias=vflag[:, 8 + rel:9 + rel], scale=1.0),
                             reads=[r_E, r_k], writes=[r_wt])
                        if partial:
                            S.op("pool", lambda wt=wt, dd=dd: P.tensor_tensor(out=wt[:], in0=wt[:], in1=masks[:, dd, :], op=ALU.mult),
                                 reads=[r_wt, r_k], writes=[r_wt])
                        S.op("pe", lambda ob=ob, vb_=vb_, kb=kb, wt=wt, o_first=o_first, o_last=o_last: T.matmul(
                            ps[ob][:], vb_[:, kb, :], wt[:], start=o_first, stop=o_last),
                             reads=[r_b, r_wt], writes=[r_ps[ob]], mark=o_last)
        for qb in range(4):
            if qb % 2:
                S.op("dve", lambda qb=qb, hp=hp: V.tensor_copy(out=hT[:, hp, qb * 512:(qb + 1) * 512], in_=ps[4 + qb][:]),
                     reads=[r_ps[4 + qb]], writes=r_hT[qb * 4:(qb + 1) * 4])
            else:
                S.op("act", lambda qb=qb, hp=hp: A.copy(out=hT[:, hp, qb * 512:(qb + 1) * 512], in_=ps[4 + qb][:]),
                     reads=[r_ps[4 + qb]], writes=r_hT[qb * 4:(qb + 1) * 4])


_PROG = {}


def _prog(kind):
    if kind not in _PROG:
        _PROG[kind] = build_moe() if kind == "moe" else build_mix(kind)
    return _PROG[kind]


def _run(kind, maps):
    res = run_bass_kernel_spmd(_prog(kind), maps, core_ids=list(range(NCORES)))
    return res.results


def kernel(x, mem, mem_ln_g, mem_ln_b, w_mem_kv, w_in_conv, conv_dw_w, conv_dw_b, conv_ln_g, conv_ln_b, w_in_sb,
           w_mix_out, ln_mix_g, ln_mix_b, w_router, b_router, w_gate_up, b_gate_up, w_down, b_down, ln_moe_g, ln_moe_b):
    f = lambda a: np.ascontiguousarray(np.asarray(a), dtype=np.float32)
    x = f(x)
    B, SEQ, _ = x.shape
    hs = [np.ascontiguousarray(x[c // 4, (c % 4) * TPC:(c % 4 + 1) * TPC]) for c in range(NCORES)]
    mems = [f(mem[c // 4]) for c in range(NCORES)]
    com_mem = dict(mem_ln_g=f(mem_ln_g)[None], mem_ln_b=f(mem_ln_b)[None], w_mem_kv=f(w_mem_kv))
    for i in range(4):
        j = i // 2
        com = dict(w_out=f(w_mix_out[i]), ln_g=f(ln_mix_g[i])[None], ln_b=f(ln_mix_b[i])[None], **com_mem)
        if i % 2 == 0:
            cvec = np.ascontiguousarray(np.concatenate([f(conv_dw_w[j]), f(conv_dw_b[j])[None], f(conv_ln_g[j])[None],
                                                        f(conv_ln_b[j])[None]], 0))
            maps = []
            for c in range(NCORES):
                halo = np.zeros((32, D), np.float32) if c % 4 == 0 else np.ascontiguousarray(hs[c - 1][-32:])
                maps.append(dict(h=hs[c], halo=halo, mem=mems[c], w_in=f(w_in_conv[j]), cvec=cvec, **com))
            r = _run("conv", maps)
        else:
            w_in = f(w_in_sb[j])
            rk = _run("kv", [dict(h=hs[c], w_in=w_in) for c in range(NCORES)])
            zk = np.zeros((MIXW, TPC), ml_dtypes.bfloat16)
            zv = np.zeros((TPC, MIXW), ml_dtypes.bfloat16)
            maps = []
            for c in range(NCORES):
                ks, vs, val = [], [], []
                for rel in range(4):
                    ok = (c % 4) - rel >= 0
                    ks.append(rk[c - rel]["kT"] if ok else zk)
                    vs.append(rk[c - rel]["v"] if ok else zv)
                    val.append(1.0 if ok else 0.0)
                maps.append(dict(h=hs[c], mem=mems[c], w_in=w_in, kT_rel=np.ascontiguousarray(np.stack(ks)),
                                 v_rel=np.ascontiguousarray(np.stack(vs)), valid=np.array([val], np.float32), **com))
            r = _run("sb", maps)
        hs = [r[c]["out"] for c in range(NCORES)]
        moe = dict(w_router=f(w_router[i]), b_router=f(b_router[i])[None], w_gate_up=f(w_gate_up[i]), b_gate_up=f(b_gate_up[i]),
                   w_down=f(w_down[i]), b_down=f(b_down[i]), ln_g=f(ln_moe_g[i])[None], ln_b=f(ln_moe_b[i])[None])
        r = _run("moe", [dict(h=hs[c], **moe) for c in range(NCORES)])
        hs = [r[c]["out"] for c in range(NCORES)]
    out = np.empty((B, SEQ, D), np.float32)
    for c in range(NCORES):
        out[c // 4, (c % 4) * TPC:(c % 4 + 1) * TPC] = hs[c]
    return out
```

```python
import numpy as np
import ml_dtypes
import concourse.bass as bass
import concourse.mybir as mybir
from concourse.bass_utils import run_bass_kernel_spmd

F32 = mybir.dt.float32
BF16 = mybir.dt.bfloat16
I32 = mybir.dt.int32
AF = mybir.ActivationFunctionType
ALU = mybir.AluOpType

D = 1024
NCORES = 8
TPC = 2048
NT = TPC // 128
NE = 32
DFF = 1024
LN_EPS = 1e-5
DN_ALPHA = (2 * 4) ** 0.25
CW = 31
MIXW = 768
MEMW = 256
NMEM = 256


class Res:
    __slots__ = ("name", "w", "r", "sem", "dcount")

    ALL = []

    def __init__(self, name):
        Res.ALL.append(self)
        self.name = name
        self.w = None
        self.r = []
        self.sem = None
        self.dcount = 0


class Sync:
    ENGS = ("pe", "act", "dve", "pool", "sp")

    def __init__(self, nc, stack):
        self.nc = nc
        self.stack = stack
        self.eng = {"pe": nc.tensor, "act": nc.scalar, "dve": nc.vector, "pool": nc.gpsimd, "sp": nc.sync}
        self.sem = {}
        self.cnt = {}
        self.pending = {}
        for e in self.ENGS:
            self.sem[e] = stack.enter_context(nc.semaphore("s_" + e))
            self.cnt[e] = 0
            self.pending[e] = False
        self.seen = {e: {} for e in self.ENGS}
        self.nsem = 0
        self.free_sems = []
        self.live_sems = []
        self.cc_sems = []
        self.gen = 0

    def _new_sem(self, name):
        if self.free_sems:
            return self.free_sems.pop()
        self.nsem += 1
        return [self.stack.enter_context(self.nc.semaphore("d%d" % self.nsem)), 0]

    def barrier(self):
        for e in ("pe", "act", "dve", "pool"):
            if self.cnt[e] > 0:
                self._wait("sp", ("eng", e, self.cnt[e]))
        for sl in self.live_sems + self.cc_sems:
            self._wait("sp", ("dma", sl[0], sl[1]))
        self.cc_sems = []
        self.gen += 1
        for e in ("pe", "act", "dve", "pool"):
            self.sem[e] = self.stack.enter_context(self.nc.semaphore("s_%s_g%d" % (e, self.gen)))
            self.cnt[e] = 0
        self.free_sems.extend(self.live_sems)
        self.live_sems = []
        self.cnt["sp"] += 1
        self.nc.sync.sem_inc(self.sem["sp"], 1)
        spk = ("eng", "sp")
        for e in self.ENGS:
            keep = self.seen[e].get(spk, 0)
            self.seen[e] = {spk: keep} if keep else {}
        for e in ("pe", "act", "dve", "pool"):
            self._wait(e, ("eng", "sp", self.cnt["sp"]))
        for r in Res.ALL:
            r.w = None
            r.r = []
            r.sem = None

    def collective(self, in_ap, out_ap, groups, reads, writes):
        self.deps("pool", reads, writes)
        tr = writes[0]
        if tr.sem is None:
            self.nsem += 1
            tr.sem = [self.stack.enter_context(self.nc.semaphore("cc%d" % self.nsem)), 0]
            self.cc_sems.append(tr.sem)
        ins = self.nc.gpsimd.collective_compute("AllGather", ALU.bypass, replica_groups=groups, ins=[in_ap], outs=[out_ap])
        tr.sem[1] += 1
        ins.then_inc(tr.sem[0], 1)
        t = ("dma", tr.sem[0], tr.sem[1])
        for w in writes:
            w.w = t
            w.r = []
        for r in reads:
            r.r.append(t)

    def _wait(self, e, ticket):
        if ticket is None:
            return
        kind, key, val = ticket
        if kind == "eng" and key == e and e == "pe":
            return
        k = (kind, id(key) if kind == "dma" else key)
        if self.seen[e].get(k, 0) >= val:
            return
        self.seen[e][k] = val
        semobj = self.sem[key] if kind == "eng" else key
        self.eng[e].wait_ge(semobj, val)

    def deps(self, e, reads, writes):
        for r in reads:
            self._wait(e, r.w)
        for w in writes:
            self._wait(e, w.w)
            for t in w.r:
                self._wait(e, t)

    def op(self, e, fn, reads=(), writes=(), mark=True):
        self.deps(e, reads, writes)
        ins = fn()
        if mark:
            self.cnt[e] += 1
            ins.then_inc(self.sem[e], 1)
            self.pending[e] = False
            t = ("eng", e, self.cnt[e])
        else:
            self.pending[e] = True
            t = ("eng", e, self.cnt[e] + 1)
        for w in writes:
            w.w = t
            w.r = []
        for r in reads:
            r.r.append(t)
            if len(r.r) > 24:
                r.r = r.r[-24:] if False else r.r
        return ins

    def dma(self, q, out, in_, reads=(), writes=(), track=None):
        self.deps(q, reads, writes)
        tr = track if track is not None else writes[0]
        if tr.sem is None:
            tr.sem = self._new_sem("d_" + tr.name)
            self.live_sems.append(tr.sem)
        ins = self.eng[q].dma_start(out=out, in_=in_)
        tr.sem[1] += 16
        ins.then_inc(tr.sem[0], 16)
        t = ("dma", tr.sem[0], tr.sem[1])
        for w in writes:
            w.w = t
            w.r = []
        if track is not None and track not in writes:
            track.w = t
        for r in reads:
            r.r.append(t)
        return ins

    def finish(self, final_res):
        for r in final_res:
            self._wait("sp", r.w)


_UID = [0]


def _mk(stack, nc, name, shape, dt):
    _UID[0] += 1
    return stack.enter_context(nc.sbuf_tensor("%s_%d" % (name, _UID[0]), shape, dt))


def _mkp(stack, nc, name, shape, dt=F32):
    return stack.enter_context(nc.psum_tensor(name, shape, dt))


def _consts(S, stack, nc):
    ident = _mk(stack, nc, "ident", [128, 128], F32)
    r_ident = Res("ident")
    S.op("pool", lambda: nc.gpsimd.memset(ident[:], 0.0), writes=[r_ident])
    S.op("pool", lambda: nc.gpsimd.affine_select(out=ident[:], in_=ident[:], pattern=[[-1, 128]],
                                                  compare_op=ALU.not_equal, fill=1.0, base=0,
                                                  channel_multiplier=1), reads=[r_ident], writes=[r_ident])
    return ident, r_ident


def _layernorm_tile(S, nc, r_t, r_res, stats, mv, rstd, gbc, bbc, r_stats, r_gb, out_t, r_out):
    for hf in range(2):
        S.op("dve", lambda hf=hf: nc.vector.bn_stats(out=stats[:, hf, :], in_=r_t[:, hf * 512:(hf + 1) * 512]),
             reads=[r_res], writes=[r_stats])
    S.op("dve", lambda: nc.vector.bn_aggr(out=mv[:], in_=stats[:].rearrange("p a b -> p (a b)")),
         reads=[r_stats], writes=[r_stats])
    S.op("act", lambda: nc.scalar.activation(out=rstd[:], in_=mv[:, 1:2], func=AF.Sqrt, bias=epsb_holder[0][:], scale=1.0),
         reads=[r_stats], writes=[r_stats])
    S.op("dve", lambda: nc.vector.reciprocal(out=rstd[:], in_=rstd[:]), reads=[r_stats], writes=[r_stats])
    S.op("dve", lambda: nc.vector.tensor_scalar(out=r_t[:], in0=r_t[:], scalar1=mv[:, 0:1], scalar2=rstd[:, 0:1],
                                                op0=ALU.subtract, op1=ALU.mult),
         reads=[r_res, r_stats], writes=[r_res])
    S.op("dve", lambda: nc.vector.tensor_tensor(out=r_t[:], in0=r_t[:], in1=gbc[:], op=ALU.mult),
         reads=[r_res, r_gb], writes=[r_res])
    S.op("dve", lambda: nc.vector.tensor_tensor(out=out_t[:], in0=r_t[:], in1=bbc[:], op=ALU.add),
         reads=[r_res, r_gb], writes=[r_out])


epsb_holder = [None]


def _eps_tile(S, stack, nc):
    epsb = _mk(stack, nc, "epsb", [128, 1], F32)
    r = Res("epsb")
    S.op("pool", lambda: nc.gpsimd.memset(epsb[:], LN_EPS), writes=[r])
    epsb_holder[0] = epsb
    epsb_holder.append(r)
    return epsb, r


def emit_moe(env, io):
    from contextlib import ExitStack
    nc, S, ps, r_ps, ident, r_ident, epsb, r_eps = env
    h_d, wr_d, br_d, wgu_d, bgu_d, wd_d, bd_d, g_d, b_d, out_d = (io[k] for k in (
        "h", "w_router", "b_router", "w_gate_up", "b_gate_up", "w_down", "b_down", "ln_g", "ln_b", "out"))
    with ExitStack() as stack:
        V, A, P, T = nc.vector, nc.scalar, nc.gpsimd, nc.tensor

        hT = _mk(stack, nc, "hT", [128, 8, TPC], BF16)
        r_hT = [Res("hT%d" % i) for i in range(NT)]
        acc = _mk(stack, nc, "acc", [128, NT, D], F32)
        r_acc = [Res("acc%d" % i) for i in range(NT)]
        actT = _mk(stack, nc, "actT", [128, 8, TPC], BF16)
        r_actT = [[Res("actT%d_%d" % (fc, b)) for b in range(4)] for fc in range(8)]
        NWG = 2
        wgu = [_mk(stack, nc, "wgu%d" % i, [128, 8, 512], BF16) for i in range(NWG)]
        r_wgu = [Res("wgu%d" % i) for i in range(NWG)]
        wdb = [_mk(stack, nc, "wd%d" % i, [128, 8, 512], BF16) for i in range(2)]
        r_wd = [Res("wd%d" % i) for i in range(2)]
        gatew = _mk(stack, nc, "gatew", [128, NT, NE], F32)
        r_gw = [Res("gw%d" % i) for i in range(NT)]
        bgu = _mk(stack, nc, "bgu", [128, 16, NE], F32)
        r_bgu = Res("bgu")
        r_bd = Res("bd_sb")
        wr32 = _mk(stack, nc, "wr32", [128, 8, NE], F32)
        r_wr = Res("wr32")
        brbc = _mk(stack, nc, "brbc", [128, NE], F32)
        r_br = Res("brbc")
        r_gb = Res("gb")
        NW = 2
        htile = [_mk(stack, nc, "htile%d" % i, [128, D], F32) for i in range(NW)]
        r_ht = [Res("htile%d" % i) for i in range(NW)]
        hT32 = [_mk(stack, nc, "hTlo_0", [128, 8, 128], BF16)] * NW
        wrs = _mk(stack, nc, "wrs", [128, 2, 8, NE], BF16)
        r_hT32 = [Res("hT32_0")] * NW
        wk_all = _mk(stack, nc, "wk_all", [128, 6, 512], F32)
        wk = [[wk_all[:, i * 3 + j, :] for j in range(3)] for i in range(2)]
        r_wk = [[Res("wk%d_%d" % (i, j)) for j in range(3)] for i in range(2)]
        gbc = wk_all[:, 0:2, :].rearrange("p a b -> p (a b)")
        bbc = wk_all[:, 2:4, :].rearrange("p a b -> p (a b)")
        bd_sb = wk_all[:NE, 4:6, :].rearrange("p a b -> p (a b)")
        r_allwk = [r for rr in r_wk for r in rr]
        small = [_mk(stack, nc, "small%d" % i, [128, 64], F32) for i in range(NW)]
        r_small = [Res("small%d" % i) for i in range(NW)]
        stats = [_mk(stack, nc, "stats%d" % i, [128, 2, 6], F32) for i in range(NW)]
        mv = [_mk(stack, nc, "mv%d" % i, [128, 2], F32) for i in range(NW)]
        rstd = [_mk(stack, nc, "rstd%d" % i, [128, 1], F32) for i in range(NW)]
        r_stats = [Res("stats%d" % i) for i in range(NW)]
        gwT = [_mk(stack, nc, "gwT%d" % i, [NE, 128], BF16) for i in range(NW)]
        r_gwT = [Res("gwT%d" % i) for i in range(NW)]

        S.dma("sp", wr32[:], wr_d.rearrange("(c p) n -> p c n", p=128), writes=[r_wr])
        S.dma("sp", brbc[:], br_d[0:1, :].to_broadcast([128, NE]), writes=[r_br])
        S.op("dve", lambda: V.tensor_copy(out=wrs[:, 0, :, :], in_=wr32[:]), reads=[r_wr], writes=[r_wr])
        S.op("dve", lambda: V.tensor_tensor(out=wrs[:, 1, :, :], in0=wr32[:], in1=wrs[:, 0, :, :], op=ALU.subtract),
             reads=[r_wr], writes=[r_wr])
        for q in range(4):
            rows, r_rows = wk[0][q % 3], r_wk[0][q % 3]
            S.dma("sp", rows[:NE, :], bgu_d[:, q * 512:(q + 1) * 512], writes=[r_rows])
            for c4 in range(4):
                ch = q * 4 + c4
                pt = ps[ch % 2]
                S.op("pe", lambda c4=c4, pt=pt, rows=rows: T.transpose(pt[:, 0:NE], rows[:NE, c4 * 128:(c4 + 1) * 128], ident[:NE, :NE]),
                     reads=[r_rows, r_ident], writes=[r_ps[ch % 2]])
                S.op("dve", lambda ch=ch, pt=pt: V.tensor_copy(out=bgu[:, ch, :], in_=pt[:, 0:NE]),
                     reads=[r_ps[ch % 2]], writes=[r_bgu])

        if DBG.get("stop") == 10:
            S.barrier()
            return
        for t in range(NT):
            w = t % NW
            S.dma("sp", htile[w][:], h_d[t * 128:(t + 1) * 128, :], writes=[r_ht[w]])
            for half in range(2):
                pb = 2 + half
                for c4 in range(4):
                    c = half * 4 + c4
                    S.op("pe", lambda c=c, c4=c4, pb=pb, w=w: T.transpose(ps[pb][:, c4 * 128:(c4 + 1) * 128],
                                                                       htile[w][:, c * 128:(c + 1) * 128], ident[:]),
                         reads=[r_ht[w], r_ident], writes=[r_ps[pb]], mark=(c4 == 3))
                S.op("dve", lambda half=half, pb=pb, t=t: V.tensor_copy(
                    out=hT[:, half * 4:(half + 1) * 4, t * 128:(t + 1) * 128],
                    in_=ps[pb][:].rearrange("p (c n) -> p c n", c=4)), reads=[r_ps[pb]], writes=[r_hT[t]])
                S.op("dve", lambda half=half, pb=pb, w=w, t=t: V.tensor_tensor(
                    out=hT32[w][:, half * 4:(half + 1) * 4, :],
                    in0=ps[pb][:].rearrange("p (c n) -> p c n", c=4),
                    in1=hT[:, half * 4:(half + 1) * 4, t * 128:(t + 1) * 128], op=ALU.subtract),
                     reads=[r_ps[pb], r_hT[t]], writes=[r_hT32[w]])
            if DBG.get("p1", 9) < 2:
                continue
            pl = 4 + (t % 2)
            k = 0
            for (lh, rh) in ((0, 0), (1, 0), (0, 1)):
                for c in range(8):
                    lhs = hT[:, c, t * 128:(t + 1) * 128] if lh == 0 else hT32[w][:, c, :]
                    S.op("pe", lambda lhs=lhs, rh=rh, c=c, pl=pl, k=k: T.matmul(ps[pl][:, 0:NE], lhs, wrs[:, rh, c, :],
                                                                         start=(k == 0), stop=(k == 23)),
                         reads=[r_hT32[w], r_hT[t], r_wr], writes=[r_ps[pl]], mark=(k == 23))
                    k += 1
            sm = small[w]
            S.op("dve", lambda pl=pl, sm=sm: V.tensor_tensor(out=sm[:, 0:NE], in0=ps[pl][:, 0:NE], in1=brbc[:], op=ALU.add),
                 reads=[r_ps[pl], r_br], writes=[r_small[w]])
            if DBG.get("p1", 9) < 3:
                continue
            S.op("dve", lambda sm=sm: V.max(out=sm[:, 32:40], in_=sm[:, 0:NE]), reads=[r_small[w]], writes=[r_small[w]])
            S.op("dve", lambda sm=sm: V.tensor_scalar(out=sm[:, 40:41], in0=sm[:, 32:33], scalar1=-1.0, scalar2=None, op0=ALU.mult),
                 reads=[r_small[w]], writes=[r_small[w]])
            S.op("act", lambda sm=sm, t=t: A.activation(out=gatew[:, t, :], in_=sm[:, 0:NE], func=AF.Exp, bias=sm[:, 40:41], scale=1.0),
                 reads=[r_small[w]], writes=[r_gw[t]])
            S.op("dve", lambda sm=sm: V.tensor_scalar(out=sm[:, 0:NE], in0=sm[:, 0:NE], scalar1=sm[:, 35:36], scalar2=None, op0=ALU.is_ge),
                 reads=[r_small[w]], writes=[r_small[w]])
            S.op("dve", lambda sm=sm, t=t: V.tensor_tensor(out=gatew[:, t, :], in0=gatew[:, t, :], in1=sm[:, 0:NE], op=ALU.mult),
                 reads=[r_small[w], r_gw[t]], writes=[r_gw[t]])
            S.op("dve", lambda sm=sm, t=t: V.reduce_sum(out=sm[:, 41:42], in_=gatew[:, t, :], axis=mybir.AxisListType.X),
                 reads=[r_gw[t]], writes=[r_small[w]])
            S.op("dve", lambda sm=sm: V.reciprocal(out=sm[:, 42:43], in_=sm[:, 41:42]), reads=[r_small[w]], writes=[r_small[w]])
            S.op("dve", lambda sm=sm, t=t: V.tensor_scalar(out=gatew[:, t, :], in0=gatew[:, t, :], scalar1=sm[:, 42:43], scalar2=None, op0=ALU.mult),
                 reads=[r_small[w], r_gw[t]], writes=[r_gw[t]])

        if DBG.get("stop") == 1:
            S.barrier()
            return
        stgw = [_mk(stack, nc, "stgw%d" % i, [128, 8, 256], F32) for i in range(2)]
        r_stgw = [Res("stgw%d" % i) for i in range(2)]
        lc = [0]

        def load_cast(dst, src_ap, r_dst):
            b = lc[0] % 2
            lc[0] += 1
            S.dma("sp", stgw[b][:], src_ap, writes=[r_stgw[b]])
            if b == 0:
                S.op("pool", lambda: P.tensor_copy(out=dst, in_=stgw[b][:]), reads=[r_stgw[b]], writes=[r_dst])
            else:
                S.op("act", lambda: A.copy(out=dst, in_=stgw[b][:]), reads=[r_stgw[b]], writes=[r_dst])

        piece_i = 0
        pp = 0
        for e in range(DBG.get("ne", NE)):
            for fp in range(4):
                wi = piece_i % NWG
                piece_i += 1
                src = wgu_d[e].rearrange("(c p) n -> p c n", p=128)
                load_cast(wgu[wi][:, :, 0:256], src[:, :, fp * 256:(fp + 1) * 256], r_wgu[wi])
                load_cast(wgu[wi][:, :, 256:512], src[:, :, DFF + fp * 256:DFF + (fp + 1) * 256], r_wgu[wi])
                for blk in range(4):
                    for j in range(2):
                        fc = fp * 2 + j
                        pg, pu = 2 * (pp % 2), 2 * (pp % 2) + 1
                        wks = wk[pp % 2]
                        rws = r_wk[pp % 2]
                        pp += 1
                        for c in range(8):
                            S.op("pe", lambda c=c, pg=pg, wi=wi, j=j, blk=blk: T.matmul(
                                ps[pg][:], wgu[wi][:, c, j * 128:(j + 1) * 128], hT[:, c, blk * 512:(blk + 1) * 512],
                                start=(c == 0), stop=(c == 7)),
                                 reads=[r_wgu[wi]] + r_hT[blk * 4:(blk + 1) * 4], writes=[r_ps[pg]], mark=(c == 7))
                        for c in range(8):
                            S.op("pe", lambda c=c, pu=pu, wi=wi, j=j, blk=blk: T.matmul(
                                ps[pu][:], wgu[wi][:, c, 256 + j * 128:256 + (j + 1) * 128], hT[:, c, blk * 512:(blk + 1) * 512],
                                start=(c == 0), stop=(c == 7)),
                                 reads=[r_wgu[wi]] + r_hT[blk * 4:(blk + 1) * 4], writes=[r_ps[pu]], mark=(c == 7))
                        gsb, sg, usb = wks
                        S.op("dve", lambda pg=pg, gsb=gsb, fc=fc, e=e: V.tensor_scalar(
                            out=gsb[:], in0=ps[pg][:], scalar1=bgu[:, fc, e:e + 1], scalar2=7.0, op0=ALU.add, op1=ALU.min),
                             reads=[r_ps[pg], r_bgu], writes=[rws[0]])
                        S.op("act", lambda gsb=gsb, sg=sg: A.activation(out=sg[:], in_=gsb[:], func=AF.Sigmoid, scale=1.702),
                             reads=[rws[0]], writes=[rws[1]])
                        S.op("dve", lambda pu=pu, usb=usb, fc=fc, e=e: V.tensor_scalar(
                            out=usb[:], in0=ps[pu][:], scalar1=bgu[:, 8 + fc, e:e + 1], scalar2=7.0, op0=ALU.add, op1=ALU.min),
                             reads=[r_ps[pu], r_bgu], writes=[rws[2]])
                        S.op("pool", lambda usb=usb: P.tensor_scalar(out=usb[:], in0=usb[:], scalar1=-7.0, scalar2=1.0,
                                                                     op0=ALU.max, op1=ALU.add),
                             reads=[rws[2]], writes=[rws[2]])
                        S.op("pool", lambda gsb=gsb, sg=sg: P.tensor_tensor(out=sg[:], in0=gsb[:], in1=sg[:], op=ALU.mult),
                             reads=[rws[0], rws[1]], writes=[rws[1]])
                        S.op("dve", lambda sg=sg, usb=usb, fc=fc, blk=blk: V.tensor_tensor(
                            out=actT[:, fc, blk * 512:(blk + 1) * 512], in0=sg[:], in1=usb[:], op=ALU.mult),
                             reads=[rws[1], rws[2]], writes=[r_actT[fc][blk]])
            srcd = wd_d[e].rearrange("(c p) n -> p c n", p=128)
            for half in range(2):
                load_cast(wdb[half][:, :, 0:256], srcd[:, :, half * 512:half * 512 + 256], r_wd[half])
                load_cast(wdb[half][:, :, 256:512], srcd[:, :, half * 512 + 256:(half + 1) * 512], r_wd[half])
            for t in range(NT):
                blk = t // 4
                for half in range(2):
                    py = 4 + 2 * (t % 2) + half
                    for fc in range(8):
                        S.op("pe", lambda fc=fc, py=py, t=t, half=half: T.matmul(
                            ps[py][:], actT[:, fc, t * 128:(t + 1) * 128], wdb[half][:, fc, :],
                            start=(fc == 0), stop=(fc == 7)),
                             reads=[r_actT[fc][blk], r_wd[half]], writes=[r_ps[py]], mark=(fc == 7))
                    if e == 0:
                        S.op("dve", lambda py=py, t=t, half=half, e=e: V.tensor_scalar(
                            out=acc[:, t, half * 512:(half + 1) * 512], in0=ps[py][:], scalar1=gatew[:, t, e:e + 1],
                            scalar2=None, op0=ALU.mult),
                             reads=[r_ps[py], r_gw[t]], writes=[r_acc[t]])
                    else:
                        S.op("dve", lambda py=py, t=t, half=half, e=e: V.scalar_tensor_tensor(
                            out=acc[:, t, half * 512:(half + 1) * 512], in0=ps[py][:], scalar=gatew[:, t, e:e + 1],
                            in1=acc[:, t, half * 512:(half + 1) * 512], op0=ALU.mult, op1=ALU.add),
                             reads=[r_ps[py], r_gw[t], r_acc[t]], writes=[r_acc[t]])

        if DBG.get("stop") == 2:
            S.barrier()
            return
        S.dma("sp", gbc, g_d[0:1, :].to_broadcast([128, D]), writes=[r_gb] + r_allwk)
        S.dma("sp", bbc, b_d[0:1, :].to_broadcast([128, D]), writes=[r_gb] + r_allwk)
        S.dma("sp", bd_sb, bd_d[:, :], writes=[r_bd] + r_allwk)
        bd_bf = hT32[0][:NE, :, :].rearrange("p a b -> p (a b)")
        S.op("dve", lambda: V.tensor_copy(out=bd_bf, in_=bd_sb), reads=[r_bd], writes=[r_bd, r_hT32[0]])
        r_outd = [Res("outd0"), Res("outd1")]
        for t in range(NT):
            w = t % NW
            S.dma("sp", htile[w][:], h_d[t * 128:(t + 1) * 128, :], writes=[r_ht[w]])
            S.op("pe", lambda t=t: T.transpose(ps[0][:NE, 0:128], gatew[:, t, :], ident[:]),
                 reads=[r_gw[t], r_ident], writes=[r_ps[0]])
            S.op("dve", lambda w=w: V.tensor_copy(out=gwT[w][:], in_=ps[0][:NE, 0:128]), reads=[r_ps[0]], writes=[r_gwT[w]])
            for half in range(2):
                pb = 2 + half
                S.op("pe", lambda w=w, pb=pb, half=half: T.matmul(ps[pb][:], gwT[w][:], bd_bf[:, half * 512:(half + 1) * 512],
                                                               start=True, stop=True),
                     reads=[r_gwT[w], r_bd], writes=[r_ps[pb]])
                S.op("dve", lambda t=t, pb=pb, half=half: V.tensor_tensor(
                    out=acc[:, t, half * 512:(half + 1) * 512], in0=acc[:, t, half * 512:(half + 1) * 512], in1=ps[pb][:], op=ALU.add),
                     reads=[r_ps[pb], r_acc[t]], writes=[r_acc[t]])
            S.op("dve", lambda t=t, w=w: V.scalar_tensor_tensor(out=acc[:, t, :], in0=htile[w][:], scalar=float(DN_ALPHA),
                                                             in1=acc[:, t, :], op0=ALU.mult, op1=ALU.add),
                 reads=[r_ht[w], r_acc[t]], writes=[r_acc[t]])
            _layernorm_tile(S, nc, acc[:, t, :], r_acc[t], stats[w], mv[w], rstd[w], gbc, bbc, r_stats[w], r_gb,
                            htile[w], r_ht[w])
            S.dma("sp", out_d[t * 128:(t + 1) * 128, :], htile[w][:], reads=[r_ht[w]], track=r_outd[w])
        S.barrier()


def emit_mix(env, kind, io):
    from contextlib import ExitStack
    nc, S, ps, r_ps, ident, r_ident, epsb, r_eps = env
    WIN = 2 * MIXW + MEMW if kind == "conv" else 3 * MIXW + MEMW
    HALO = 32 if kind == "conv" else 0
    h_d, win_d, mem_d, mg_d, mb_d, wkv_d, wout_d, g_d, b_d, out_d = (io[k] for k in (
        "h", "w_in", "mem", "mem_ln_g", "mem_ln_b", "w_mem_kv", "w_out", "ln_g", "ln_b", "out"))
    GROUPS = [[0, 1, 2, 3], [4, 5, 6, 7]]
    if kind == "conv":
        cvec_d, sel_d, halo_in, halo_g = io["cvec"], io["sel"], io["halo_in"], io["halo_g"]
    else:
        flags_d, kT_own, v_own, kT_all, v_all = io["flags"], io["kT_own"], io["v_own"], io["kT_all"], io["v_all"]

    with ExitStack() as stack:
        V, A, P, T = nc.vector, nc.scalar, nc.gpsimd, nc.tensor
        oneb = _mk(stack, nc, "oneb", [128, 1], F32)
        r_c = Res("consts")
        S.op("pool", lambda: P.memset(oneb[:], 1.0), writes=[r_c])

        TW = HALO + TPC
        hT = _mk(stack, nc, "hT", [128, 8, TW], BF16)
        r_hT = [Res("hT%d" % i) for i in range(NT)]
        r_halo = Res("hThalo")
        wbig = _mk(stack, nc, "wbig", [128, 8, WIN], BF16)
        r_w = Res("wbig")
        htile = [_mk(stack, nc, "htile%d" % i, [128, D], F32) for i in range(2)]
        r_ht = [Res("htile%d" % i) for i in range(2)]

        S.dma("pool", wbig[:], win_d.rearrange("(c p) n -> p c n", p=128), writes=[r_w])

        def load_transpose(src_ap, nrows, col0, w, res):
            S.dma("sp", htile[w][:nrows, :], src_ap, writes=[r_ht[w]])
            for half in range(2):
                pb = 6 + half
                for c4 in range(4):
                    c = half * 4 + c4
                    S.op("pe", lambda c=c, c4=c4, pb=pb: T.transpose(ps[pb][:, c4 * 128:c4 * 128 + nrows],
                                                                   htile[w][:nrows, c * 128:(c + 1) * 128], ident[:nrows, :nrows]),
                         reads=[r_ht[w], r_ident], writes=[r_ps[pb]], mark=(c4 == 3))
                S.op("dve" if half == 0 else "act",
                     (lambda half=half, pb=pb: V.tensor_copy(
                         out=hT[:, half * 4:(half + 1) * 4, col0:col0 + nrows],
                         in_=ps[pb][:].rearrange("p (c n) -> p c n", c=4)[:, :, 0:nrows])) if half == 0 else
                     (lambda half=half, pb=pb: A.copy(
                         out=hT[:, half * 4:(half + 1) * 4, col0:col0 + nrows],
                         in_=ps[pb][:].rearrange("p (c n) -> p c n", c=4)[:, :, 0:nrows])),
                     reads=[r_ps[pb]], writes=[res])

        if kind == "conv":
            r_hin, r_hg = Res("halo_in"), Res("halo_g")
            S.dma("sp", halo_in[:, :], h_d[TPC - 32:TPC, :], writes=[r_hin])
            if DBG.get("nocc"):
                for rr in range(4):
                    S.dma("sp", halo_g[rr * 32:(rr + 1) * 32, :], halo_in[:, :], reads=[r_hin], writes=[r_hg])
            else:
                S.collective(halo_in.opt(), halo_g.opt(), GROUPS, reads=[r_hin], writes=[r_hg])
            S.dma("sp", htile[1][:], halo_g[:, :], reads=[r_hg], writes=[r_ht[1]])
            selt = _mk(stack, nc, "selt", [128, 32], F32)
            r_sel = Res("selt")
            S.dma("sp", selt[:], sel_d[:, :], writes=[r_sel])
            for c in range(8):
                pb = 6 + (c % 2)
                S.op("pe", lambda c=c, pb=pb: T.matmul(ps[pb][:, 0:32], htile[1][:, c * 128:(c + 1) * 128], selt[:], start=True, stop=True),
                     reads=[r_ht[1], r_sel], writes=[r_ps[pb]])
                S.op("dve", lambda c=c, pb=pb: V.tensor_copy(out=hT[:, c, 0:32], in_=ps[pb][:, 0:32]), reads=[r_ps[pb]], writes=[r_halo])
        for t in range(NT):
            load_transpose(h_d[t * 128:(t + 1) * 128, :], 128, HALO + t * 128, t % 2, r_hT[t])

        def inproj(pb, col_w, cols, ncols, reads):
            for c in range(8):
                S.op("pe", lambda c=c: T.matmul(ps[pb][:, 0:ncols], wbig[:, c, col_w:col_w + 128], hT[:, c, cols:cols + ncols],
                                                start=(c == 0), stop=(c == 7)),
                     reads=[r_w] + reads, writes=[r_ps[pb]], mark=(c == 7))

        if kind == "sb":
            stg = [_mk(stack, nc, "stg%d" % i, [128, MIXW], BF16) for i in range(2)]
            r_stg = [Res("stg%d" % i) for i in range(2)]
            r_sd = [Res("stgd0"), Res("stgd1")]
            n = 0
            for blk in range(4):
                for kc in range(6):
                    pb = n % 2
                    n += 1
                    inproj(pb, MIXW + kc * 128, blk * 512, 512, r_hT[blk * 4:(blk + 1) * 4])
                    sb = stg[pb]
                    S.op("dve", lambda sb=sb, pb=pb: V.tensor_copy(out=sb[:, 0:512], in_=ps[pb][:]), reads=[r_ps[pb]], writes=[r_stg[pb]])
                    S.dma("sp", kT_own[kc // 2][(kc % 2) * 128:(kc % 2 + 1) * 128, blk * 512:(blk + 1) * 512], sb[:, 0:512], reads=[r_stg[pb]], track=r_sd[pb])
            for t in range(NT):
                sb = stg[t % 2]
                for part, (c0, cn) in enumerate(((0, 512), (512, 256))):
                    pb = 2 + part
                    for c in range(8):
                        S.op("pe", lambda c=c, pb=pb, c0=c0, cn=cn, t=t: T.matmul(
                            ps[pb][:, 0:cn], hT[:, c, t * 128:(t + 1) * 128], wbig[:, c, 2 * MIXW + c0:2 * MIXW + c0 + cn],
                            start=(c == 0), stop=(c == 7)), reads=[r_w, r_hT[t]], writes=[r_ps[pb]], mark=(c == 7))
                    S.op("dve", lambda sb=sb, pb=pb, c0=c0, cn=cn: V.tensor_copy(out=sb[:, c0:c0 + cn], in_=ps[pb][:, 0:cn]),
                         reads=[r_ps[pb]], writes=[r_stg[t % 2]])
                S.dma("sp", v_own[t // 4][(t % 4) * 128:(t % 4 + 1) * 128, :], sb[:], reads=[r_stg[t % 2]], track=r_sd[t % 2])
            r_ka, r_va = Res("kT_all"), Res("v_all")
            for p_ in range(3):
                S.collective(kT_own[p_].opt(), kT_all[p_].opt(), GROUPS, reads=r_sd, writes=[r_ka])
            for p_ in range(4):
                S.collective(v_own[p_].opt(), v_all[p_].opt(), GROUPS, reads=r_sd, writes=[r_va])

        wkv = _mk(stack, nc, "wkv", [128, 8, 2 * MEMW], BF16)
        r_wkv = Res("wkv")
        S.dma("pool", wkv[:], wkv_d.rearrange("(c p) n -> p c n", p=128), writes=[r_wkv])
        gbc = _mk(stack, nc, "gbc", [128, D], F32)
        bbc = _mk(stack, nc, "bbc", [128, D], F32)
        r_gb = Res("gb")
        S.dma("sp", gbc[:], mg_d[0:1, :].to_broadcast([128, D]), writes=[r_gb])
        S.dma("sp", bbc[:], mb_d[0:1, :].to_broadcast([128, D]), writes=[r_gb])
        stats = [_mk(stack, nc, "stats%d" % i, [128, 2, 6], F32) for i in range(2)]
        mv = [_mk(stack, nc, "mv%d" % i, [128, 2], F32) for i in range(2)]
        rstd = [_mk(stack, nc, "rstd%d" % i, [128, 1], F32) for i in range(2)]
        r_stats = [Res("stats%d" % i) for i in range(2)]
        memT = _mk(stack, nc, "memT", [128, 8, NMEM], BF16)
        r_memT = Res("memT")
        wkf = [_mk(stack, nc, "wkf%d" % i, [128, D], F32) for i in range(2)]
        r_wkf = [Res("wkf%d" % i) for i in range(2)]
        for mt in range(2):
            S.dma("sp", htile[mt][:], mem_d[mt * 128:(mt + 1) * 128, :], writes=[r_ht[mt]])
            _layernorm_tile(S, nc, htile[mt], r_ht[mt], stats[mt], mv[mt], rstd[mt], gbc, bbc, r_stats[mt], r_gb, wkf[mt], r_wkf[mt])
            for half in range(2):
                pb = 6 + half
                for c4 in range(4):
                    c = half * 4 + c4
                    S.op("pe", lambda c=c, c4=c4, pb=pb, mt=mt: T.transpose(ps[pb][:, c4 * 128:(c4 + 1) * 128],
                                                                        wkf[mt][:, c * 128:(c + 1) * 128], ident[:]),
                         reads=[r_wkf[mt], r_ident], writes=[r_ps[pb]], mark=(c4 == 3))
                S.op("dve", lambda half=half, pb=pb, mt=mt: V.tensor_copy(
                    out=memT[:, half * 4:(half + 1) * 4, mt * 128:(mt + 1) * 128],
                    in_=ps[pb][:].rearrange("p (c n) -> p c n", c=4)), reads=[r_ps[pb]], writes=[r_memT])
        kTp = _mk(stack, nc, "kTp", [128, 4, NMEM], BF16)
        Vp = _mk(stack, nc, "Vp", [128, 2, 4, 128], BF16)
        onesp = _mk(stack, nc, "onesp", [128, 2, 128], BF16)
        r_mkv = Res("memkv")
        S.op("pool", lambda: P.memset(kTp[:], 0.0), writes=[r_mkv])
        S.op("pool", lambda: P.memset(Vp[:], 0.0), writes=[r_mkv])
        S.op("pool", lambda: P.memset(onesp[:], 0.0), writes=[r_mkv])
        S.op("pool", lambda: P.memset(onesp[:, 0, 0:64], 1.0), writes=[r_mkv])
        S.op("pool", lambda: P.memset(onesp[:, 1, 64:128], 1.0), writes=[r_mkv])
        for hp in range(2):
            for c in range(8):
                S.op("pe", lambda c=c, hp=hp: T.matmul(ps[0][:, 0:NMEM], wkv[:, c, hp * 128:(hp + 1) * 128], memT[:, c, :],
                                                       start=(c == 0), stop=(c == 7)),
                     reads=[r_wkv, r_memT], writes=[r_ps[0]], mark=(c == 7))
            for par in range(2):
                S.op("dve", lambda hp=hp, par=par: V.tensor_copy(out=kTp[par * 64:(par + 1) * 64, hp * 2 + par, :],
                                                                 in_=ps[0][par * 64:(par + 1) * 64, 0:NMEM]),
                     reads=[r_ps[0]], writes=[r_mkv])
        for mc in range(2):
            for c in range(8):
                S.op("pe", lambda c=c, mc=mc: T.matmul(ps[1][:, 0:MEMW], memT[:, c, mc * 128:(mc + 1) * 128], wkv[:, c, MEMW:2 * MEMW],
                                                       start=(c == 0), stop=(c == 7)),
                     reads=[r_wkv, r_memT], writes=[r_ps[1]], mark=(c == 7))
            for hh_ in range(4):
                par = hh_ % 2
                S.op("dve", lambda mc=mc, hh_=hh_, par=par: V.tensor_copy(out=Vp[:, mc, hh_, par * 64:(par + 1) * 64],
                                                                          in_=ps[1][:, hh_ * 64:(hh_ + 1) * 64]),
                     reads=[r_ps[1]], writes=[r_mkv])

        qmT = _mk(stack, nc, "qmT", [128, 2, TPC], BF16)
        r_qm = [Res("qm%d" % b) for b in range(4)]
        wk = [[_mk(stack, nc, "wk%d_%d" % (i, j), [128, 512], F32) for j in range(3)] for i in range(2)]
        r_wk = [[Res("wk%d_%d" % (i, j)) for j in range(3)] for i in range(2)]
        qm_col = 2 * MIXW if kind == "conv" else 3 * MIXW
        n = 0
        for blk in range(4):
            for j in range(2):
                pb = n % 2
                n += 1
                inproj(pb, qm_col + j * 128, HALO + blk * 512, 512, r_hT[blk * 4:(blk + 1) * 4])
                S.op("act", lambda pb=pb, j=j, blk=blk: A.copy(out=qmT[:, j, blk * 512:(blk + 1) * 512], in_=ps[pb][:]),
                     reads=[r_ps[pb]], writes=[r_qm[blk]])

        if kind == "conv":
            cw = _mk(stack, nc, "cw", [128, 6, 34], F32)
            r_cw = Res("cw")
            S.dma("sp", wkf[0][:34, 0:MIXW], cvec_d[:, :], writes=[r_wkf[0]])
            for cc in range(6):
                S.op("pe", lambda cc=cc: T.transpose(ps[2][:, 0:34], wkf[0][:34, cc * 128:(cc + 1) * 128], ident[:34, :34]),
                     reads=[r_wkf[0], r_ident], writes=[r_ps[2]])
                S.op("dve", lambda cc=cc: V.tensor_copy(out=cw[:, cc, :], in_=ps[2][:, 0:34]), reads=[r_ps[2]], writes=[r_cw])
            hh = [_mk(stack, nc, "hh%d" % i, [128, TW], F32) for i in range(2)]
            r_hh = [Res("hh%d" % i) for i in range(2)]
            co = _mk(stack, nc, "co", [128, 6, TPC], F32)
            r_co = [Res("co%d" % i) for i in range(6)]
            for cc in range(6):
                hb, r_hb = hh[cc % 2], r_hh[cc % 2]
                for bi in range(5):
                    cols, ncols = (0, 32) if bi == 0 else (HALO + (bi - 1) * 512, 512)
                    rd = [r_halo] if bi == 0 else r_hT[(bi - 1) * 4:bi * 4]
                    pa, pg = 2 * (n % 2), 2 * (n % 2) + 1
                    n += 1
                    inproj(pa, cc * 128, cols, ncols, rd)
                    inproj(pg, MIXW + cc * 128, cols, ncols, rd)
                    sg, r_sg = wk[n % 2][0], r_wk[n % 2][0]
                    S.op("act", lambda pg=pg, sg=sg, ncols=ncols: A.activation(out=sg[:, 0:ncols], in_=ps[pg][:, 0:ncols], func=AF.Sigmoid),
                         reads=[r_ps[pg]], writes=[r_sg])
                    S.op("dve", lambda pa=pa, sg=sg, hb=hb, cols=cols, ncols=ncols: V.tensor_tensor(
                        out=hb[:, cols:cols + ncols], in0=ps[pa][:, 0:ncols], in1=sg[:, 0:ncols], op=ALU.mult),
                         reads=[r_ps[pa], r_sg], writes=[r_hb])
                S.op("dve", lambda cc=cc, hb=hb: V.tensor_scalar(out=co[:, cc, :], in0=hb[:, 2:2 + TPC], scalar1=cw[:, cc, 0:1],
                                                                 scalar2=cw[:, cc, 31:32], op0=ALU.mult, op1=ALU.add),
                     reads=[r_hb, r_cw], writes=[r_co[cc]])
                for k in range(1, CW):
                    S.op("dve", lambda cc=cc, hb=hb, k=k: V.scalar_tensor_tensor(
                        out=co[:, cc, :], in0=hb[:, 2 + k:2 + k + TPC], scalar=cw[:, cc, k:k + 1], in1=co[:, cc, :],
                        op0=ALU.mult, op1=ALU.add), reads=[r_hb, r_cw, r_co[cc]], writes=[r_co[cc]])
            onesm = _mk(stack, nc, "onesm", [128, 128], F32)
            S.op("pool", lambda: P.memset(onesm[:], 1.0 / MIXW), writes=[r_c])
            for blk in range(4):
                bs = slice(blk * 512, (blk + 1) * 512)
                for cc in range(6):
                    S.op("pe", lambda cc=cc, bs=bs: T.matmul(ps[2][:], onesm[:], co[:, cc, bs], start=(cc == 0), stop=(cc == 5)),
                         reads=[r_c, r_co[cc]], writes=[r_ps[2]], mark=(cc == 5))
                for cc in range(6):
                    sq, r_sq = wk[cc % 2][1], r_wk[cc % 2][1]
                    S.op("act", lambda cc=cc, bs=bs, sq=sq: A.activation(out=sq[:], in_=co[:, cc, bs], func=AF.Square),
                         reads=[r_co[cc]], writes=[r_sq])
                    S.op("pe", lambda cc=cc, sq=sq: T.matmul(ps[3][:], onesm[:], sq[:], start=(cc == 0), stop=(cc == 5)),
                         reads=[r_c, r_sq], writes=[r_ps[3]])
                mean, r_mean = wk[0][2], r_wk[0][2]
                rs, r_rs = wk[1][2], r_wk[1][2]
                S.op("act", lambda mean=mean: A.copy(out=mean[:], in_=ps[2][:]), reads=[r_ps[2]], writes=[r_mean])
                S.op("dve", lambda mean=mean, rs=rs: V.tensor_tensor(out=rs[:], in0=mean[:], in1=mean[:], op=ALU.mult),
                     reads=[r_mean], writes=[r_rs])
                S.op("dve", lambda rs=rs: V.tensor_tensor(out=rs[:], in0=ps[3][:], in1=rs[:], op=ALU.subtract),
                     reads=[r_ps[3], r_rs], writes=[r_rs])
                S.op("act", lambda rs=rs: A.activation(out=rs[:], in_=rs[:], func=AF.Sqrt, bias=epsb[:], scale=1.0),
                     reads=[r_rs, r_eps], writes=[r_rs])
                S.op("dve", lambda rs=rs: V.reciprocal(out=rs[:], in_=rs[:]), reads=[r_rs], writes=[r_rs])
                for cc in range(6):
                    t1, r_t1 = wk[cc % 2][0], r_wk[cc % 2][0]
                    S.op("dve", lambda cc=cc, bs=bs, t1=t1, mean=mean: V.tensor_tensor(out=t1[:], in0=co[:, cc, bs], in1=mean[:], op=ALU.subtract),
                         reads=[r_co[cc], r_mean], writes=[r_t1])
                    S.op("pool", lambda t1=t1, rs=rs: P.tensor_tensor(out=t1[:], in0=t1[:], in1=rs[:], op=ALU.mult),
                         reads=[r_t1, r_rs], writes=[r_t1])
                    S.op("act", lambda cc=cc, t1=t1, blk=blk: A.activation(
                        out=hT[:, cc, HALO + blk * 512:HALO + (blk + 1) * 512], in_=t1[:], func=AF.Silu,
                        scale=cw[:, cc, 32:33], bias=cw[:, cc, 33:34]),
                         reads=[r_t1, r_cw], writes=r_hT[blk * 4:(blk + 1) * 4] + ([r_halo] if blk == 0 else []))
        else:
            _sb_attention(S, stack, nc, locals())

        PT = [_mk(stack, nc, "PT%d" % i, [128, 512], BF16) for i in range(4)]
        r_PT = [Res("PT%d" % i) for i in range(4)]
        n = 0
        for blk in range(4):
            for hp in range(2):
                for par in range(2):
                    hd = hp * 2 + par
                    for mc in range(2):
                        pb = 2 + (n % 2)
                        pt, r_pt = PT[n % 4], r_PT[n % 4]
                        n += 1
                        S.op("pe", lambda hd=hd, mc=mc, pb=pb, hp=hp, blk=blk: T.matmul(
                            ps[pb][:], kTp[:, hd, mc * 128:(mc + 1) * 128], qmT[:, hp, blk * 512:(blk + 1) * 512], start=True, stop=True),
                             reads=[r_mkv, r_qm[blk]], writes=[r_ps[pb]])
                        S.op("act", lambda pb=pb, pt=pt: A.activation(out=pt[:], in_=ps[pb][:], func=AF.Exp, scale=0.125),
                             reads=[r_ps[pb]], writes=[r_pt])
                        first, last = (par == 0 and mc == 0), (par == 1 and mc == 1)
                        S.op("pe", lambda hd=hd, mc=mc, pt=pt, first=first, last=last: T.matmul(
                            ps[4][:], Vp[:, mc, hd, :], pt[:], start=first, stop=last),
                             reads=[r_mkv, r_pt], writes=[r_ps[4]], mark=last)
                        S.op("pe", lambda par=par, pt=pt, first=first, last=last: T.matmul(
                            ps[5][:], onesp[:, par, :], pt[:], start=first, stop=last),
                             reads=[r_mkv, r_pt], writes=[r_ps[5]], mark=last)
                rd, r_rd = wk[hp][1], r_wk[hp][1]
                S.op("dve", lambda rd=rd: V.reciprocal(out=rd[:], in_=ps[5][:]), reads=[r_ps[5]], writes=[r_rd])
                S.op("dve", lambda rd=rd, hp=hp, blk=blk: V.tensor_tensor(
                    out=hT[:, 6 + hp, HALO + blk * 512:HALO + (blk + 1) * 512], in0=ps[4][:], in1=rd[:], op=ALU.mult),
                     reads=[r_ps[4], r_rd], writes=r_hT[blk * 4:(blk + 1) * 4])

        S.dma("pool", wbig[:, :, 0:D], wout_d.rearrange("(c p) n -> p c n", p=128), writes=[r_w])
        S.dma("sp", gbc[:], g_d[0:1, :].to_broadcast([128, D]), writes=[r_gb])
        S.dma("sp", bbc[:], b_d[0:1, :].to_broadcast([128, D]), writes=[r_gb])
        r_outd = [Res("outd0"), Res("outd1")]
        for t in range(NT):
            w = t % 2
            S.dma("sp", htile[w][:], h_d[t * 128:(t + 1) * 128, :], writes=[r_ht[w]])
            for half in range(2):
                pb = 6 + half
                for c in range(8):
                    S.op("pe", lambda c=c, pb=pb, t=t, half=half: T.matmul(
                        ps[pb][:], hT[:, c, HALO + t * 128:HALO + (t + 1) * 128], wbig[:, c, half * 512:(half + 1) * 512],
                        start=(c == 0), stop=(c == 7)), reads=[r_hT[t], r_w], writes=[r_ps[pb]], mark=(c == 7))
                S.op("dve", lambda w=w, pb=pb, half=half: V.scalar_tensor_tensor(
                    out=wkf[w][:, half * 512:(half + 1) * 512], in0=htile[w][:, half * 512:(half + 1) * 512], scalar=float(DN_ALPHA),
                    in1=ps[pb][:], op0=ALU.mult, op1=ALU.add), reads=[r_ht[w], r_ps[pb]], writes=[r_wkf[w]])
            _layernorm_tile(S, nc, wkf[w], r_wkf[w], stats[w], mv[w], rstd[w], gbc, bbc, r_stats[w], r_gb, htile[w], r_ht[w])
            S.dma("sp", out_d[t * 128:(t + 1) * 128, :], htile[w][:], reads=[r_ht[w]], track=r_outd[w])
        S.barrier()


def _sb_attention(S, stack, nc, L):
    V, A, P, T = nc.vector, nc.scalar, nc.gpsimd, nc.tensor
    hT, r_hT, ps, r_ps, wk, r_wk, inproj = L["hT"], L["r_hT"], L["ps"], L["r_ps"], L["wk"], L["r_wk"], L["inproj"]
    flags_d, oneb, r_c, r_w = L["flags_d"], L["oneb"], L["r_c"], L["r_w"]
    kT_all, v_all, r_ka, r_va = L["kT_all"], L["v_all"], L["r_ka"], L["r_va"]
    qT = _mk(stack, nc, "qT", [128, 6, TPC], BF16)
    r_q = [Res("q%d" % b) for b in range(4)]
    n = 0
    for blk in range(4):
        for qc in range(6):
            pb = n % 2
            n += 1
            inproj(pb, qc * 128, blk * 512, 512, r_hT[blk * 4:(blk + 1) * 4])
            if n % 2:
                S.op("dve", lambda pb=pb, qc=qc, blk=blk: V.tensor_copy(out=qT[:, qc, blk * 512:(blk + 1) * 512], in_=ps[pb][:]),
                     reads=[r_ps[pb]], writes=[r_q[blk]])
            else:
                S.op("act", lambda pb=pb, qc=qc, blk=blk: A.copy(out=qT[:, qc, blk * 512:(blk + 1) * 512], in_=ps[pb][:]),
                     reads=[r_ps[pb]], writes=[r_q[blk]])
    U = _mk(stack, nc, "Umat", [128, 128], BF16)
    ones = _mk(stack, nc, "onesbf", [128, 128], BF16)
    Mk = _mk(stack, nc, "Mk", [128, 4, 4, 512], BF16)
    vflag = _mk(stack, nc, "vflag", [128, 28], F32)
    r_k = Res("sbconst")
    S.op("pool", lambda: P.memset(ones[:], 1.0), writes=[r_k])
    S.op("pool", lambda: P.memset(U[:], 1.0), writes=[r_k])
    S.op("pool", lambda: P.affine_select(out=U[:], in_=U[:], pattern=[[-1, 128]], compare_op=ALU.is_gt, fill=0.0, base=0,
                                         channel_multiplier=1), reads=[r_k], writes=[r_k])
    S.op("pool", lambda: P.memset(Mk[:], 1.0), writes=[r_k])
    for a in range(4):
        for dd in range(4):
            S.op("pool", lambda a=a, dd=dd: P.affine_select(out=Mk[:, a, dd, :], in_=Mk[:, a, dd, :], pattern=[[1, 512]],
                                                            compare_op=ALU.is_gt, fill=0.0, base=-dd * 128, channel_multiplier=-1),
                 reads=[r_k], writes=[r_k])
    S.dma("sp", vflag[:, 0:8], flags_d[0:1, :].to_broadcast([128, 8]), writes=[r_k])
    S.op("dve", lambda: V.tensor_tensor(out=vflag[:, 8:12], in0=vflag[:, 0:4], in1=vflag[:, 4:8], op=ALU.add), reads=[r_k], writes=[r_k])
    S.op("dve", lambda: V.tensor_scalar(out=vflag[:, 12:16], in0=vflag[:, 8:12], scalar1=-1.0, scalar2=None, op0=ALU.mult),
         reads=[r_k], writes=[r_k])
    S.op("dve", lambda: V.tensor_scalar(out=vflag[:, 16:20], in0=vflag[:, 8:12], scalar1=-1.0, scalar2=30000.0, op0=ALU.add, op1=ALU.mult),
         reads=[r_k], writes=[r_k])
    S.op("dve", lambda: V.tensor_scalar(out=vflag[:, 20:24], in0=vflag[:, 0:4], scalar1=-1.0, scalar2=None, op0=ALU.mult),
         reads=[r_k], writes=[r_k])
    S.op("dve", lambda: V.tensor_scalar(out=vflag[:, 24:28], in0=vflag[:, 0:4], scalar1=-1.0, scalar2=30000.0, op0=ALU.add, op1=ALU.mult),
         reads=[r_k], writes=[r_k])
    for a in range(4):
        for dd in range(4):
            S.op("dve", lambda a=a, dd=dd: V.tensor_scalar(out=Mk[:, a, dd, :], in0=Mk[:, a, dd, :], scalar1=vflag[:, 4 + a:5 + a],
                                                           scalar2=vflag[:, a:a + 1], op0=ALU.mult, op1=ALU.add),
                 reads=[r_k], writes=[r_k])
    kpad = [[_mk(stack, nc, "kpad%d_%d" % (p_, b_), [128, TPC], BF16) for b_ in range(1)] for p_ in range(2)]
    vpad = [[_mk(stack, nc, "vpad%d_%d" % (p_, b_), [128, 16, 128], BF16) for b_ in range(1)] for p_ in range(2)]
    r_kv = [[Res("kv%d_%d" % (p_, b_)) for b_ in range(1)] for p_ in range(2)]
    for p_ in range(2):
        for b_ in range(1):
            S.op("pool", lambda p_=p_, b_=b_: P.memset(kpad[p_][b_][:], 0.0), writes=[r_kv[p_][b_]])
            S.op("pool", lambda p_=p_, b_=b_: P.memset(vpad[p_][b_][:], 0.0), writes=[r_kv[p_][b_]])
    Sacc = [_mk(stack, nc, "Sacc%d" % i, [128, 512], BF16) for i in range(4)]
    r_S = [Res("Sacc%d" % i) for i in range(4)]
    Lt = [_mk(stack, nc, "Lt%d" % i, [128, 512], BF16) for i in range(2)]
    Wt = [_mk(stack, nc, "Wt%d" % i, [128, 512], BF16) for i in range(2)]
    r_Lt = [Res("Lt%d" % i) for i in range(2)]
    r_Wt = [Res("Wt%d" % i) for i in range(2)]
    n = 0
    for hp in range(6):
        for par in range(2):
            hd = hp * 2 + par
            for a in (3, 2, 1, 0):
                kb_, vb_, r_b = kpad[par][0], vpad[par][0], r_kv[par][0]
                S.dma("sp", kb_[par * 64:(par + 1) * 64, :],
                      kT_all[hd // 4][a * 256 + (hd % 4) * 64:a * 256 + (hd % 4 + 1) * 64, :], reads=[r_ka], writes=[r_b])
                for p_ in range(4):
                    S.dma("sp", vb_[:, 4 * p_:4 * p_ + 4, par * 64:(par + 1) * 64],
                          v_all[p_][a * 512:(a + 1) * 512, hd * 64:(hd + 1) * 64].rearrange("(kb p) d -> p kb d", p=128),
                          reads=[r_va], writes=[r_b])
                for qb in range(4):
                    for kb in range(15, -1, -1):
                        first_sweep = (a == 3 and kb == 15)
                        o_first = first_sweep and par == 0
                        o_last = (par == 1 and a == 0 and kb == 0)
                        dd = kb - 4 * qb
                        partial = 0 <= dd <= 3
                        fneg, fbias = (12 + a, 16 + a) if dd < 0 else (20 + a, 24 + a)
                        i2 = n % 2
                        n += 1
                        zb, tb, ob = i2, 2 + i2, 4 + qb
                        E, r_E = wk[i2][0], r_wk[i2][0]
                        Sp, r_Sp = wk[i2][1], r_wk[i2][1]
                        lt, r_lt, wt, r_wt = Lt[i2], r_Lt[i2], Wt[i2], r_Wt[i2]
                        qs = slice(qb * 512, (qb + 1) * 512)
                        S.op("pe", lambda zb=zb, kb_=kb_, kb=kb, hp=hp, qs=qs: T.matmul(
                            ps[zb][:], kb_[:, kb * 128:(kb + 1) * 128], qT[:, hp, qs], start=True, stop=True),
                             reads=[r_b, r_q[qb]], writes=[r_ps[zb]])
                        S.op("act", lambda zb=zb, E=E: A.activation(out=E[:], in_=ps[zb][:], func=AF.Exp, scale=0.125),
                             reads=[r_ps[zb]], writes=[r_E])
                        S.op("act", lambda E=E, Sp=Sp: A.activation(out=Sp[:], in_=E[:], func=AF.Ln, bias=oneb[:], scale=1.0),
                             reads=[r_E, r_c], writes=[r_Sp])
                        if partial:
                            S.op("dve", lambda Sp=Sp, lt=lt, a=a, dd=dd: V.scalar_tensor_tensor(
                                out=lt[:], in0=Sp[:], scalar=-1.0, in1=Mk[:, a, dd, :], op0=ALU.mult, op1=ALU.mult),
                                 reads=[r_Sp, r_k], writes=[r_lt])
                        else:
                            S.op("dve", lambda Sp=Sp, lt=lt, fneg=fneg: V.tensor_scalar(
                                out=lt[:], in0=Sp[:], scalar1=vflag[:, fneg:fneg + 1], scalar2=None, op0=ALU.mult),
                                 reads=[r_Sp, r_k], writes=[r_lt])
                        S.op("pe", lambda tb=tb, lt=lt, fs=first_sweep: T.matmul(ps[tb][:], U[:], lt[:], start=True, stop=fs),
                             reads=[r_k, r_lt], writes=[r_ps[tb]], mark=first_sweep)
                        if not first_sweep:
                            S.op("pe", lambda tb=tb, qb=qb: T.matmul(ps[tb][:], ones[:], Sacc[qb][:], start=False, stop=True),
                                 reads=[r_k, r_S[qb]], writes=[r_ps[tb]])
                            S.op("pool", lambda qb=qb, lt=lt: P.tensor_tensor(out=Sacc[qb][:], in0=Sacc[qb][:], in1=lt[:], op=ALU.add),
                                 reads=[r_S[qb], r_lt], writes=[r_S[qb]])
                        else:
                            S.op("pool", lambda qb=qb, lt=lt: P.tensor_copy(out=Sacc[qb][:], in_=lt[:]),
                                 reads=[r_lt], writes=[r_S[qb]])
                        S.op("dve", lambda zb=zb, E=E, Sp=Sp: V.scalar_tensor_tensor(
                            out=E[:], in0=ps[zb][:], scalar=0.125, in1=Sp[:], op0=ALU.mult, op1=ALU.subtract),
                             reads=[r_ps[zb], r_Sp], writes=[r_E])
                        S.op("dve", lambda tb=tb, E=E: V.tensor_tensor(out=E[:], in0=E[:], in1=ps[tb][:], op=ALU.add),
                             reads=[r_E, r_ps[tb]], writes=[r_E])
                        if partial:
                            S.op("act", lambda E=E, wt=wt: A.activation(out=wt[:], in_=E[:], func=AF.Exp),
                                 reads=[r_E], writes=[r_wt])
                            S.op("pool", lambda wt=wt, a=a, dd=dd: P.tensor_tensor(out=wt[:], in0=wt[:], in1=Mk[:, a, dd, :], op=ALU.mult),
                                 reads=[r_wt, r_k], writes=[r_wt])
                        else:
                            S.op("act", lambda E=E, wt=wt, fbias=fbias: A.activation(out=wt[:], in_=E[:], func=AF.Exp,
                                                                                   bias=vflag[:, fbias:fbias + 1], scale=1.0),
                                 reads=[r_E, r_k], writes=[r_wt])
                        S.op("pe", lambda ob=ob, vb_=vb_, kb=kb, wt=wt, o_first=o_first, o_last=o_last: T.matmul(
                            ps[ob][:], vb_[:, kb, :], wt[:], start=o_first, stop=o_last),
                             reads=[r_b, r_wt], writes=[r_ps[ob]])
        for qb in range(4):
            if qb % 2:
                S.op("dve", lambda qb=qb, hp=hp: V.tensor_copy(out=hT[:, hp, qb * 512:(qb + 1) * 512], in_=ps[4 + qb][:]),
                     reads=[r_ps[4 + qb]], writes=r_hT[qb * 4:(qb + 1) * 4])
            else:
                S.op("act", lambda qb=qb, hp=hp: A.copy(out=hT[:, hp, qb * 512:(qb + 1) * 512], in_=ps[4 + qb][:]),
                     reads=[r_ps[4 + qb]], writes=r_hT[qb * 4:(qb + 1) * 4])


NLAYER = 4
DBG = {}


def build_fused(nlayer=NLAYER, with_moe=True, kinds=None):
    from contextlib import ExitStack
    Res.ALL = []
    nc = bass.Bass("TRN2", target_bir_lowering=False)
    din = lambda n, s, dt=F32: nc.dram_tensor(n, s, dt, kind="ExternalInput").ap()
    x_d = din("x", [TPC, D])
    mem_d = din("mem", [NMEM, D])
    mg_d, mb_d = din("mem_ln_g", [1, D]), din("mem_ln_b", [1, D])
    wkv_d = din("w_mem_kv", [D, 2 * MEMW])
    winc_d = din("w_in_conv", [2, D, 2 * MIXW + MEMW])
    cvec_d = din("cvec", [2, 34, MIXW])
    wins_d = din("w_in_sb", [2, D, 3 * MIXW + MEMW])
    wout_d = din("w_mix_out", [NLAYER, D, D])
    lmg_d, lmb_d = din("ln_mix_g", [NLAYER, D]), din("ln_mix_b", [NLAYER, D])
    if with_moe:
        wr_d, br_d = din("w_router", [nlayer, D, NE]), din("b_router", [nlayer, NE])
        wgu_d, bgu_d = din("w_gate_up", [nlayer, DBG.get("ne", NE), D, 2 * DFF]), din("b_gate_up", [nlayer, NE, 2 * DFF])
        wd_d, bd_d = din("w_down", [nlayer, DBG.get("ne", NE), DFF, D]), din("b_down", [nlayer, NE, D])
        log_d, lob_d = din("ln_moe_g", [nlayer, D]), din("ln_moe_b", [nlayer, D])
    sel_d = din("sel", [128, 32])
    flags_d = din("flags", [1, 8])
    out_d = nc.dram_tensor("out", [TPC, D], F32, kind="ExternalOutput").ap()
    hbuf = [nc.dram_tensor("hbuf%d" % i, [TPC, D], F32).ap() for i in range(2 * NLAYER - 1)]
    halo_in = [nc.dram_tensor("halo_in%d" % i, [32, D], F32).ap() for i in range(2)]
    halo_g = [nc.dram_tensor("halo_g%d" % i, [128, D], F32).ap() for i in range(2)]
    kT_own = [[nc.dram_tensor("kT_own%d_%d" % (i, p), [256, TPC], BF16).ap() for p in range(3)] for i in range(2)]
    v_own = [[nc.dram_tensor("v_own%d_%d" % (i, p), [512, MIXW], BF16).ap() for p in range(4)] for i in range(2)]
    kT_all = [[nc.dram_tensor("kT_all%d_%d" % (i, p), [4 * 256, TPC], BF16).ap() for p in range(3)] for i in range(2)]
    v_all = [[nc.dram_tensor("v_all%d_%d" % (i, p), [4 * 512, MIXW], BF16).ap() for p in range(4)] for i in range(2)]
    with ExitStack() as stack:
        S = Sync(nc, stack)
        ident, r_ident = _consts(S, stack, nc)
        epsb, r_eps = _eps_tile(S, stack, nc)
        ps = [_mkp(stack, nc, "ps%d" % i, [128, 512]) for i in range(8)]
        r_ps = [Res("ps%d" % i) for i in range(8)]
        env = (nc, S, ps, r_ps, ident, r_ident, epsb, r_eps)
        cur = x_d
        nb = 0
        for i in range(nlayer):
            j = i // 2
            nxt = hbuf[nb] if (with_moe or i < nlayer - 1) else out_d
            nb += 1
            io = dict(h=cur, mem=mem_d, mem_ln_g=mg_d, mem_ln_b=mb_d, w_mem_kv=wkv_d, w_out=wout_d[i],
                      ln_g=lmg_d[i:i + 1, :], ln_b=lmb_d[i:i + 1, :], out=nxt)
            if kinds and kinds[i] == "none":
                nxt = cur
            elif (kinds[i] if kinds else ("conv" if i % 2 == 0 else "sb")) == "conv":
                io.update(w_in=winc_d[j], cvec=cvec_d[j], sel=sel_d, halo_in=halo_in[j], halo_g=halo_g[j])
                emit_mix(env, "conv", io)
            else:
                io.update(w_in=wins_d[j], flags=flags_d, kT_own=kT_own[j], v_own=v_own[j], kT_all=kT_all[j], v_all=v_all[j])
                emit_mix(env, "sb", io)
            cur = nxt
            if not with_moe:
                continue
            if i == nlayer - 1:
                nxt = out_d
            else:
                nxt = hbuf[nb]
                nb += 1
            emit_moe(env, dict(h=(x_d if DBG.get('moe_in_x') else cur), w_router=wr_d[i], b_router=br_d[i:i + 1, :], w_gate_up=wgu_d[i], b_gate_up=bgu_d[i],
                               w_down=wd_d[i], b_down=bd_d[i], ln_g=log_d[i:i + 1, :], ln_b=lob_d[i:i + 1, :], out=nxt))
            cur = nxt
    return nc


_PROG = {}


def kernel(x, mem, mem_ln_g, mem_ln_b, w_mem_kv, w_in_conv, conv_dw_w, conv_dw_b, conv_ln_g, conv_ln_b, w_in_sb,
           w_mix_out, ln_mix_g, ln_mix_b, w_router, b_router, w_gate_up, b_gate_up, w_down, b_down, ln_moe_g, ln_moe_b):
    f = lambda a: np.ascontiguousarray(np.asarray(a), dtype=np.float32)
    x = f(x)
    B, SEQ, _ = x.shape
    if "fused" not in _PROG:
        _PROG["fused"] = build_fused()
    cvec = np.ascontiguousarray(np.concatenate([f(conv_dw_w), f(conv_dw_b)[:, None, :], f(conv_ln_g)[:, None, :],
                                                f(conv_ln_b)[:, None, :]], axis=1))
    shared = dict(mem_ln_g=f(mem_ln_g)[None], mem_ln_b=f(mem_ln_b)[None], w_mem_kv=f(w_mem_kv), w_in_conv=f(w_in_conv), cvec=cvec,
                  w_in_sb=f(w_in_sb), w_mix_out=f(w_mix_out), ln_mix_g=f(ln_mix_g), ln_mix_b=f(ln_mix_b), w_router=f(w_router),
                  b_router=f(b_router), w_gate_up=f(w_gate_up), b_gate_up=f(b_gate_up), w_down=f(w_down), b_down=f(b_down),
                  ln_moe_g=f(ln_moe_g), ln_moe_b=f(ln_moe_b))
    maps = []
    for c in range(NCORES):
        jc = c % 4
        sel = np.zeros((128, 32), np.float32)
        if jc > 0:
            sel[(jc - 1) * 32 + np.arange(32), np.arange(32)] = 1.0
        flags = np.zeros((1, 8), np.float32)
        flags[0, :jc] = 1.0
        flags[0, 4 + jc] = 1.0
        maps.append(dict(x=np.ascontiguousarray(x[c // 4, jc * TPC:(jc + 1) * TPC]), mem=f(mem[c // 4]), sel=sel, flags=flags, **shared))
    res = run_bass_kernel_spmd(_PROG["fused"], maps, core_ids=list(range(NCORES)))
    out = np.empty((B, SEQ, D), np.float32)
    for c in range(NCORES):
        out[c // 4, (c % 4) * TPC:(c % 4 + 1) * TPC] = res.results[c]["out"]
    return out
```
